# Optimizing a Trainium2 kernel written in Bass

```python
import jax
import jax.numpy as jnp
from jax import lax
import numpy as np

D_MODEL = 1024
BATCH = 8
SEQ = 2048
DEPTH = 2

N_MIXERS = 2
N_HEADS = 16
HEAD_DIM = D_MODEL // N_HEADS
KV_LATENT = D_MODEL // 4
IDX_HEADS = 8
IDX_DIM = 64
TOPK_MAX = 256
Q_BLOCK = 128
POOL_WINDOWS = (2, 4, 8, 16)
POOL_GROUPS = len(POOL_WINDOWS)
POOL_GROUP_DIM = D_MODEL // POOL_GROUPS
D_FF = -(-(8 * D_MODEL) // (3 * 256)) * 256
Q_COLS = N_HEADS * HEAD_DIM
QIDX_COLS = IDX_HEADS * IDX_DIM
A_IN_DIM = Q_COLS + KV_LATENT + QIDX_COLS + IDX_DIM + IDX_HEADS
N_A_LAYERS = (DEPTH + N_MIXERS - 1) // N_MIXERS
N_B_LAYERS = DEPTH // N_MIXERS
DEEPNORM_ALPHA = (2 * DEPTH) ** 0.25
DEEPNORM_BETA = (8 * DEPTH) ** -0.25
LN_EPS = 1e-5
RMS_EPS = 1e-6

kernel_name = 'hybrid_dsa_pool_deepnorm'


def alibi_slopes(n_heads):
    return jnp.exp2(-8.0 * jnp.arange(1, n_heads + 1, dtype=jnp.float32) / n_heads)


def layer_norm(x, g, b):
    xf = x.astype(jnp.float32)
    mu = jnp.mean(xf, axis=-1, keepdims=True)
    var = jnp.mean(jnp.square(xf - mu), axis=-1, keepdims=True)
    y = (xf - mu) * lax.rsqrt(var + LN_EPS)
    return (y * g.astype(jnp.float32) + b.astype(jnp.float32)).astype(x.dtype)


def rms_norm(x, g):
    xf = x.astype(jnp.float32)
    y = xf * lax.rsqrt(jnp.mean(jnp.square(xf), axis=-1, keepdims=True) + RMS_EPS)
    return (y * g.astype(jnp.float32)).astype(x.dtype)


def to_blocks(t, n_blocks):
    t = t.reshape((t.shape[0], n_blocks, Q_BLOCK) + t.shape[2:])
    return jnp.swapaxes(t, 0, 1)


def sparse_indexer_attention(h, w_in, w_uk, w_uv, kv_norm_g, w_o):
    bsz, seq, _ = h.shape
    topk = min(TOPK_MAX, seq // 4)
    n_blocks = seq // Q_BLOCK
    proj = h @ w_in
    offs = [Q_COLS, Q_COLS + KV_LATENT, Q_COLS + KV_LATENT + QIDX_COLS,
            Q_COLS + KV_LATENT + QIDX_COLS + IDX_DIM]
    q, c_kv, q_idx, k_idx, w_idx = jnp.split(proj, offs, axis=-1)
    q = q.reshape(bsz, seq, N_HEADS, HEAD_DIM)
    c_kv = rms_norm(c_kv, kv_norm_g)
    q_lat = jnp.einsum('blhd,hcd->blhc', q, w_uk) * (HEAD_DIM ** -0.5)
    q_idx = q_idx.reshape(bsz, seq, IDX_HEADS, IDX_DIM) * (IDX_DIM ** -0.5)
    w_idx = w_idx * (IDX_HEADS ** -0.5)
    slopes = alibi_slopes(N_HEADS)
    key_pos = jnp.arange(seq, dtype=jnp.int32)
    q_pos = key_pos.reshape(n_blocks, Q_BLOCK)

    def block(args):
        ql, qi, wi, qp = args
        rel = jax.nn.relu(jnp.einsum('bqhd,bsd->bqhs', qi, k_idx))
        score = jnp.einsum('bqh,bqhs->bqs', wi, rel).astype(jnp.float32)
        causal = key_pos[None, :] <= qp[:, None]
        score = jnp.where(causal[None], score, -jnp.inf)
        _, sel = lax.top_k(score, topk)
        valid = sel <= qp[None, :, None]
        c_sel = jax.vmap(lambda cb, ib: cb[ib])(c_kv, sel)
        logits = jnp.einsum('bqhc,bqkc->bhqk', ql, c_sel).astype(jnp.float32)
        dist = (qp[None, :, None] - sel).astype(jnp.float32)
        logits = logits - slopes[None, :, None, None] * dist[:, None]
        logits = jnp.where(valid[:, None], logits, -jnp.inf)
        p = jax.nn.softmax(logits, axis=-1).astype(c_sel.dtype)
        return jnp.einsum('bhqk,bqkc->bqhc', p, c_sel)

    o_lat = lax.map(block, (to_blocks(q_lat, n_blocks), to_blocks(q_idx, n_blocks),
                            to_blocks(w_idx, n_blocks), q_pos))
    o_lat = jnp.swapaxes(o_lat, 0, 1).reshape(bsz, seq, N_HEADS, KV_LATENT)
    o = jnp.einsum('blhc,hcd->blhd', o_lat, w_uv).reshape(bsz, seq, Q_COLS)
    return o @ w_o


def multiscale_pool_mixer(h, w_in, w_grp, scale, w_o):
    bsz, seq, _ = h.shape
    u = (h @ w_in).reshape(bsz, seq, POOL_GROUPS, POOL_GROUP_DIM)
    uf = u.astype(jnp.float32)
    cs = jnp.concatenate([jnp.zeros_like(uf[:, :1]), jnp.cumsum(uf, axis=1)], axis=1)
    end = jnp.arange(1, seq + 1, dtype=jnp.int32)[:, None]
    win = jnp.array(POOL_WINDOWS, dtype=jnp.int32)[None, :]
    start = jnp.maximum(end - win, 0)
    count = (end - start).astype(jnp.float32)
    g_idx = jnp.arange(POOL_GROUPS, dtype=jnp.int32)[None, :]
    mean = (cs[:, 1:] - cs[:, start, g_idx]) / count[None, :, :, None]
    pooled = (mean - uf).astype(h.dtype)
    y = jnp.einsum('blgc,gcd->blgd', pooled, w_grp).reshape(bsz, seq, D_MODEL) * scale
    return y @ w_o


def swiglu_ffn(h, w_gu, w_down):
    gate, up = jnp.split(h @ w_gu, 2, axis=-1)
    return (jax.nn.silu(gate) * up) @ w_down


def setup_inputs(seed: int = 0) -> dict:
    key = jax.random.key(seed)
    ks = jax.random.split(key, 17)
    nrm = jax.random.normal
    f32 = jnp.float32
    d = D_MODEL
    return {
        'x': nrm(ks[0], (BATCH, SEQ, d), f32),
        'a_w_in': nrm(ks[1], (N_A_LAYERS, d, A_IN_DIM), f32) * d ** -0.5,
        'a_w_uk': nrm(ks[2], (N_A_LAYERS, N_HEADS, KV_LATENT, HEAD_DIM), f32) * KV_LATENT ** -0.5,
        'a_w_uv': nrm(ks[3], (N_A_LAYERS, N_HEADS, KV_LATENT, HEAD_DIM), f32) * KV_LATENT ** -0.5,
        'a_kv_norm_g': 1.0 + 0.02 * nrm(ks[4], (N_A_LAYERS, KV_LATENT), f32),
        'a_w_o': nrm(ks[5], (N_A_LAYERS, Q_COLS, d), f32) * (Q_COLS ** -0.5) * DEEPNORM_BETA,
        'b_w_in': nrm(ks[6], (N_B_LAYERS, d, d), f32) * d ** -0.5,
        'b_w_grp': nrm(ks[7], (N_B_LAYERS, POOL_GROUPS, POOL_GROUP_DIM, POOL_GROUP_DIM), f32) * POOL_GROUP_DIM ** -0.5,
        'b_scale': 1.0 + 0.1 * nrm(ks[8], (N_B_LAYERS, d), f32),
        'b_w_o': nrm(ks[9], (N_B_LAYERS, d, d), f32) * (d ** -0.5) * DEEPNORM_BETA,
        'f_w_gu': nrm(ks[10], (DEPTH, d, 2 * D_FF), f32) * d ** -0.5,
        'f_w_down': nrm(ks[11], (DEPTH, D_FF, d), f32) * (D_FF ** -0.5) * DEEPNORM_BETA,
        'ln_mix_g': 1.0 + 0.02 * nrm(ks[12], (DEPTH, d), f32),
        'ln_mix_b': 0.02 * nrm(ks[13], (DEPTH, d), f32),
        'ln_ffn_g': 1.0 + 0.02 * nrm(ks[14], (DEPTH, d), f32),
        'ln_ffn_b': 0.02 * nrm(ks[15], (DEPTH, d), f32),
    }


def reference(x, a_w_in, a_w_uk, a_w_uv, a_kv_norm_g, a_w_o, b_w_in, b_w_grp, b_scale, b_w_o,
              f_w_gu, f_w_down, ln_mix_g, ln_mix_b, ln_ffn_g, ln_ffn_b):
    h = x
    for i in range(DEPTH):
        j = i // N_MIXERS
        if i % N_MIXERS == 0:
            mix = sparse_indexer_attention(h, a_w_in[j], a_w_uk[j], a_w_uv[j], a_kv_norm_g[j], a_w_o[j])
        else:
            mix = multiscale_pool_mixer(h, b_w_in[j], b_w_grp[j], b_scale[j], b_w_o[j])
        h = layer_norm(DEEPNORM_ALPHA * h + mix, ln_mix_g[i], ln_mix_b[i])
        h = layer_norm(DEEPNORM_ALPHA * h + swiglu_ffn(h, f_w_gu[i], f_w_down[i]), ln_ffn_g[i], ln_ffn_b[i])
    return h
```

```python
import contextlib
import numpy as np
import ml_dtypes
import concourse.bass as bass
import concourse.mybir as mybir
from concourse.bass_utils import run_bass_kernel_spmd

F32 = mybir.dt.float32
BF16 = mybir.dt.bfloat16
ALU = mybir.AluOpType
AF = mybir.ActivationFunctionType
AX = mybir.AxisListType

L = 2048
D = 1024
NT = 16
NB = 4
NC8 = 8
DFF = 2816
NJ = 22
ALPHA = 4.0 ** 0.25
WINS = (2, 4, 8, 16)
NIT = 12
DEBUG = False
USE_SWDGE = False


class Op:
    __slots__ = ("eng", "fn", "deps", "ddeps", "idx", "dma", "dsem", "dval", "inc", "cnt", "waits", "dwaits")

    def __init__(self, eng, fn, deps, ddeps, idx, dma):
        self.eng, self.fn, self.deps, self.ddeps, self.idx, self.dma = eng, fn, deps, ddeps, idx, dma
        self.dsem = None
        self.dval = 0
        self.inc = False
        self.cnt = 0
        self.waits = []
        self.dwaits = []


class Prog:
    ENGS = ("pe", "act", "dve", "pool", "sp")
    RING = 8

    def __init__(self, nc, stack):
        self.nc = nc
        self.sems = {e: stack.enter_context(nc.semaphore("s_" + e)) for e in self.ENGS}
        self.dsems = {e: [stack.enter_context(nc.semaphore("d_%s%d" % (e, i))) for i in range(self.RING)]
                      for e in ("sp", "pool")}
        self.ndma = {e: 0 for e in ("sp", "pool")}
        self.count = {e: 0 for e in self.ENGS}
        self.reset_phase()

    def reset_phase(self):
        self.ops = {e: [] for e in self.ENGS}
        self.last_w = {}
        self.readers = {}
        self.phase_dma = {}

    def add(self, eng, fn, reads=(), writes=(), dma=False):
        writes = list(writes) + [t for t in reads if isinstance(t, tuple) and t[0] == "ps" and t not in writes]
        deps, ddeps = {}, {}

        def dep(ev):
            if ev[0] == "d":
                k = (ev[1], ev[2])
                if ddeps.get(k, 0) < ev[3]:
                    ddeps[k] = ev[3]
            else:
                if deps.get(ev[0], -1) < ev[1]:
                    deps[ev[0]] = ev[1]
        for t in reads:
            if t in self.last_w:
                dep(self.last_w[t])
        for t in writes:
            if t in self.last_w:
                dep(self.last_w[t])
            for r in self.readers.get(t, ()):
                dep(r)
        idx = len(self.ops[eng])
        op = Op(eng, fn, deps, ddeps, idx, dma)
        if dma:
            n = self.ndma[eng]
            self.ndma[eng] = n + 1
            slot = n % self.RING
            op.dsem = (eng, slot)
            op.dval = 16 * (n // self.RING + 1)
            if n >= self.RING:
                k = (eng, slot)
                if ddeps.get(k, 0) < op.dval - 16:
                    ddeps[k] = op.dval - 16
            ev = ("d", eng, slot, op.dval)
            self.phase_dma[(eng, slot)] = op.dval
        else:
            ev = (eng, idx)
        self.ops[eng].append(op)
        for t in reads:
            self.readers.setdefault(t, []).append(ev)
        for t in writes:
            self.last_w[t] = ev
            self.readers[t] = []
        return op

    def emit(self):
        nc = self.nc
        last = {}
        for e in self.ENGS:
            for op in reversed(self.ops[e]):
                if not op.dma:
                    last[e] = op.idx
                    break
        for e in self.ENGS:
            deps = {e2: i for e2, i in last.items() if not (e2 == e and e in ("pe", "sp"))}
            self.ops[e].append(Op(e, lambda h: h.nop(), deps, dict(self.phase_dma), len(self.ops[e]), False))
        for e in self.ENGS:
            waited, dwaited = {}, {}
            for op in self.ops[e]:
                for se, si in op.deps.items():
                    if se == "pe" and e == "pe":
                        continue
                    if waited.get(se, -1) < si:
                        waited[se] = si
                        op.waits.append((se, si))
                        self.ops[se][si].inc = True
                for k, v in op.ddeps.items():
                    if dwaited.get(k, 0) < v:
                        dwaited[k] = v
                        op.dwaits.append((k, v))
        for e in self.ENGS:
            c = self.count[e]
            for op in self.ops[e]:
                if op.inc:
                    c += 1
                op.cnt = c
            self.count[e] = c
        ops, sems, dsems = self.ops, self.sems, self.dsems

        def run(e, h):
            for op in ops[e]:
                for se, si in op.waits:
                    h.wait_ge(sems[se], ops[se][si].cnt)
                for (q, slot), v in op.dwaits:
                    h.wait_ge(dsems[q][slot], v)
                ins = op.fn(h)
                if op.dma:
                    ins.then_inc(dsems[op.dsem[0]][op.dsem[1]], 16)
                elif op.inc:
                    ins.then_inc(sems[e], 1)
        with nc.Block() as block:
            @block.tensor
            def _(h):
                run("pe", h)

            @block.scalar
            def _(h):
                run("act", h)

            @block.vector
            def _(h):
                run("dve", h)

            @block.gpsimd
            def _(h):
                run("pool", h)

            @block.sync
            def _(h):
                run("sp", h)
        self.reset_phase()


def build_nc(debug=False, stop=99):
    hcount = 0
    nc = bass.Bass("TRN2", target_bir_lowering=False)

    def din(name, shape, dt=F32):
        return nc.dram_tensor(name, list(shape), dt, kind="ExternalInput").ap()
    x_d = din("x", [L, D])
    w_in_d = din("w_in_t", [15, 128, 8, 128])
    w_widx_d = din("w_widx", [128, 8, 8])
    w_uk_d = din("w_uk_t", [128, 2, 1024])
    w_uv_d = din("w_uv_t", [128, 2, 1024])
    kvg_d = din("kvg", [128, 2])
    a_wo_d = din("a_wo_t", [8, 128, 8, 128])
    b_win_d = din("b_win_t", [8, 128, 8, 128])
    b_wgrp_d = din("b_wgrp_t", [4, 128, 2, 256])
    b_scale_d = din("b_scale_t", [128, 8])
    b_wo_d = din("b_wo_t", [8, 128, 8, 128])
    w_gu_d = din("w_gu_t", [2, 44, 128, 8, 128])
    w_dn_d = din("w_dn_t", [2, 8, 128, NJ, 128])
    lnp_d = din("lnp", [128, 2, 4, 8])
    ident_d = din("ident", [128, 128])
    identb_d = din("identb", [128, 128], BF16)
    posrows_d = din("posrows", [8, L], BF16)
    qcoef_d = din("qcoef", [16, 8, L], BF16)
    caus_add_d = din("caus_add", [128, 128])
    causT_d = din("causT", [128, 128], BF16)
    invc_d = din("invc", [128, 4, 16])
    out_d = nc.dram_tensor("out", [L, D], F32, kind="ExternalOutput").ap()
    if debug:
        dbg_d = nc.dram_tensor("dbg", [6, 128, 8, L], F32, kind="ExternalOutput").ap()

    with contextlib.ExitStack() as st:
        P = Prog(nc, st)

        def sb(name, shape, dt):
            return st.enter_context(nc.sbuf_tensor(name, list(shape), dt))
        ident = sb("ident_sb", [128, 128], F32)
        identb = sb("identb_sb", [128, 128], BF16)
        onesb = sb("onesb", [128, 128], BF16)
        lnp = sb("lnp_sb", [128, 2, 4, 8], F32)
        kvg = sb("kvg_sb", [128, 2], F32)
        bscale = sb("bscale_sb", [128, 8], F32)
        caus_add = sb("caus_add_sb", [128, 128], F32)
        causT = sb("causT_sb", [128, 128], BF16)
        invc = sb("invc_sb", [128, 4, 16], F32)
        widx_w = sb("widx_w", [128, 8, 8], BF16)
        wuk = sb("wuk_sb", [128, 2, 1024], BF16)
        wuv = sb("wuv_sb", [128, 2, 1024], BF16)
        cst = sb("cst", [128, 8], F32)
        widx_sb = sb("widx_sb", [128, 16, 8], F32)
        sc = sb("sc", [128, 32], F32)
        rden = sb("rden", [128, 2, 4], F32)
        ARENA_B = 196 * 1024
        arena = sb("arena", [128, ARENA_B // 2], BF16)
        ps = st.enter_context(nc.psum_tensor("ps", [128, 8, 512], F32))

        def view(off, dt, shape):
            n = int(np.prod(shape))
            nbytes = n * (4 if dt == F32 else 2)
            assert off % 4 == 0 and off + nbytes <= ARENA_B, (off, nbytes)
            ap = arena[:, off // 2:(off + nbytes) // 2]
            if dt == F32:
                ap = ap.bitcast(F32)
            if len(shape) == 2:
                ap = ap.rearrange("p (a b) -> p a b", a=shape[0])
            elif len(shape) == 3:
                ap = ap.rearrange("p (a b c) -> p a b c", a=shape[0], b=shape[1])
            return ap
        K = 1024

        def psb(bank):
            return ps[:, bank, :].bitcast(BF16)

        def mm(out, lhsT, rhs, start, stop, reads, writes, skip=False):
            P.add("pe", lambda h: h.matmul(out, lhsT, rhs, start=start, stop=stop, skip_group_check=skip),
                  reads, writes)

        def tr(out, in_, idn, reads, writes):
            P.add("pe", lambda h: h.transpose(out, in_, idn), reads, writes)

        def act(out, in_, func, reads, writes, bias=None, scale=None):
            kw = {}
            if bias is not None:
                kw["bias"] = bias
            if scale is not None:
                kw["scale"] = scale
            P.add("act", lambda h: h.activation(out=out, in_=in_, func=func, **kw), reads, writes)

        def tt(eng, out, in0, in1, op, reads, writes):
            P.add(eng, lambda h: h.tensor_tensor(out=out, in0=in0, in1=in1, op=op), reads, writes)

        def ts(eng, out, in0, s1, s2, op0, op1, reads, writes, accum=None):
            if op1 is None:
                P.add(eng, lambda h: h.tensor_scalar(out=out, in0=in0, scalar1=s1, scalar2=None, op0=op0),
                      reads, writes)
            elif accum is None:
                P.add(eng, lambda h: h.tensor_scalar(out=out, in0=in0, scalar1=s1, scalar2=s2, op0=op0, op1=op1),
                      reads, writes)
            else:
                P.add(eng, lambda h: h.tensor_scalar(out=out, in0=in0, scalar1=s1, scalar2=s2, op0=op0, op1=op1,
                                                     accum_out=accum), reads, writes)

        def stt(eng, out, in0, scalar, in1, op0, op1, reads, writes):
            P.add(eng, lambda h: h.scalar_tensor_tensor(out=out, in0=in0, scalar=scalar, in1=in1, op0=op0, op1=op1),
                  reads, writes)

        def cp(eng, out, in_, reads, writes):
            P.add(eng, lambda h: h.tensor_copy(out=out, in_=in_), reads, writes)

        def dma(q, out, in_, reads, writes):
            if q == "pool":
                P.add(q, lambda h: h.dma_start(out=out, in_=in_, max_dma_last_dim=4096), reads, writes, dma=True)
            else:
                P.add(q, lambda h: h.dma_start(out=out, in_=in_), reads, writes, dma=True)

        def wload(dst, src, dst_tok, stage, stage_toks, ceng="pool"):
            if USE_SWDGE:
                dma("pool", dst, src, (), [dst_tok])
            else:
                dma("sp", stage, src, (), list(stage_toks))
                if ceng == "act":
                    act(dst, stage, AF.Copy, list(stage_toks), [dst_tok])
                else:
                    cp("pool", dst, stage, list(stage_toks), [dst_tok])

        def memset(eng, ap, val, writes):
            P.add(eng, lambda h: h.memset(ap, val), (), writes)

        def cols(tb):
            return slice(tb * 512, (tb + 1) * 512)

        dbg_n = [0]
        phase_n = [0]

        class _Stop(Exception):
            pass

        def emit_phase():
            P.emit()
            phase_n[0] += 1
            if phase_n[0] >= stop:
                raise _Stop()

        def snapshot(hT):
            if debug:
                k = dbg_n[0]
                dbg_n[0] += 1
                dma("sp", dbg_d[k], hT, [("hT", c, tb) for c in range(8) for tb in range(4)], [("dbg", k)])

        try:
            dma("sp", ident[:], ident_d, (), ["ident"])
            dma("sp", identb[:], identb_d, (), ["identb"])
            dma("sp", lnp[:], lnp_d, (), ["lnp"])
            dma("sp", kvg[:], kvg_d, (), ["kvg"])
            dma("sp", bscale[:], b_scale_d, (), ["bscale"])
            dma("sp", caus_add[:], caus_add_d, (), ["caus_add"])
            dma("sp", causT[:], causT_d, (), ["causT"])
            dma("sp", invc[:], invc_d, (), ["invc"])
            wload(widx_w[:], w_widx_d, "widx_w", view(72 * 1024, F32, [8, 8]), ["stg_widx"])
            wload(wuk[:], w_uk_d, "wuk", view(80 * 1024, F32, [2, 1024]), ["stg_wuk"])
            wload(wuv[:], w_uv_d, "wuv", view(88 * 1024, F32, [2, 1024]), ["stg_wuv"])
            memset("pool", onesb[:], 1.0, ["onesb"])
            memset("pool", cst[:, 0:1], 1e-6, ["cst"])
            memset("pool", cst[:, 1:2], 1e-5, ["cst"])
            memset("pool", cst[:, 2:3], -1e29, ["cst"])

            hTb_old = view(0, BF16, [8, L])
            Qpair = view(32 * K, BF16, [8, L])
            oT = view(64 * K, BF16, [8, L])
            MOFF = [0, 4, 12, 24]
            maskT = view(96 * K, BF16, [40, 512])
            c_kvT = view(136 * K, BF16, [2, L])
            c_raw = view(144 * K, F32, [2, L])
            csq = view(160 * K, BF16, [2, L])
            k_idxT = view(168 * K, BF16, [L])
            q_idxT = view(172 * K, BF16, [4, L])
            wst = [view(188 * K + i * 2 * K, BF16, [8, 128]) for i in range(2)]
            xs = [view(64 * K + i * 4 * K, F32, [D]) for i in range(2)]
            stgA = [view(96 * K + i * 4 * K, F32, [8, 128]) for i in range(2)]

            for tti in range(NT):
                b = tti % 2
                dma("sp", xs[b], x_d[tti * 128:(tti + 1) * 128, :], (), [("xs", b)])
                for c in range(8):
                    tr(ps[:, 2 * b + c // 4, (c % 4) * 128:(c % 4 + 1) * 128], xs[b][:, c * 128:(c + 1) * 128], ident[:],
                       [("xs", b), "ident"], [("ps", 2 * b + c // 4)])
                act(hTb_old[:, :, tti * 128:(tti + 1) * 128],
                    ps[:, 2 * b:2 * b + 2, :].rearrange("p a (c t) -> p (a c) t", t=128), AF.Copy,
                    [("ps", 2 * b), ("ps", 2 * b + 1)], [("hTbo", tti // 4)])

            bank_rr = [0]

            def nbank(lo=0, n=4):
                b = lo + bank_rr[0] % n
                bank_rr[0] += 1
                return b
            for tix in range(15):
                wb = tix % 2
                wload(wst[wb], w_in_d[tix], ("wst", wb), stgA[wb], [("stgA", wb)])
                for tb in range(NB):
                    bk = nbank()
                    for c in range(8):
                        mm(ps[:, bk, :], wst[wb][:, c, :], hTb_old[:, c, cols(tb)], c == 0, c == 7,
                           [("wst", wb), ("hTbo", tb)], [("ps", bk)])
                    if tix < 8:
                        act(Qpair[:, tix, cols(tb)], ps[:, bk, :], AF.Copy, [("ps", bk)], [("Qpair", tix, tb)], scale=0.125)
                    elif tix < 10:
                        cp("dve", c_raw[:, tix - 8, cols(tb)], ps[:, bk, :], [("ps", bk)], [("c_raw", tb)])
                        act(csq[:, tix - 8, cols(tb)], ps[:, bk, :], AF.Square, [("ps", bk)], [("csq", tb)])
                    elif tix < 14:
                        act(q_idxT[:, tix - 10, cols(tb)], ps[:, bk, :], AF.Copy, [("ps", bk)], [("q_idxT",)])
                    else:
                        cp("dve", k_idxT[:, cols(tb)], ps[:, bk, :], [("ps", bk)], [("k_idxT",)])
            for tti in range(NT):
                for c in range(8):
                    mm(ps[:, 4, tti * 8:(tti + 1) * 8], hTb_old[:, c, tti * 128:(tti + 1) * 128], widx_w[:, c, :],
                       c == 0, c == 7, [("hTbo", tti // 4), "widx_w"], [("ps", 4)], skip=True)
            cp("dve", widx_sb[:], ps[:, 4, 0:128].rearrange("p (a b) -> p a b", a=16), [("ps", 4)], ["widx_sb"])

            sd_a = xs[0][:, 0:512]
            rstd_a = xs[1][:, 0:512]
            for tb in range(NB):
                bk = 5
                for cc in range(2):
                    mm(ps[:, bk, :], onesb[:], csq[:, cc, cols(tb)], cc == 0, cc == 1, ["onesb", ("csq", tb)], [("ps", bk)])
                act(sd_a, ps[:, bk, :], AF.Sqrt, [("ps", bk), "cst"], [("xs", 0)],
                    bias=cst[:, 0:1], scale=1.0 / 256)
                P.add("dve", lambda h: h.reciprocal(out=rstd_a, in_=sd_a), [("xs", 0)], [("xs", 1)])
                for cc in range(2):
                    stt("dve", c_kvT[:, cc, cols(tb)], c_raw[:, cc, cols(tb)], kvg[:, cc:cc + 1], rstd_a, ALU.mult, ALU.mult,
                        [("c_raw", tb), "kvg", ("xs", 1)], [("c_kvT", tb)])
            emit_phase()

            acc = [view(i * 8 * K, F32, [L]) for i in range(4)]
            A3O = 144 * K
            rbuf = [view(A3O + i * 2 * K, F32, [512]) for i in range(4)]
            junk = view(A3O + 8 * K, BF16, [L])
            mask_qs = [view(A3O + 12 * K + i * 4 * K, BF16, [L]) for i in range(2)]
            tbank = [0]
            F_LO, F_MX, F_W, F_HW, F_MID, F_CNT, F_STEP = range(7)

            def scf(f, c0=0, c1=4):
                return sc[:, f * 4 + c0:f * 4 + c1]
            rcount = 0
            for QB in range(NB):
                for jq in range(4):
                    qt = 4 * QB + jq
                    n = (qt + 1) * 128
                    a = acc[jq]
                    atok = ("acc", jq)
                    nsb = (n + 511) // 512
                    for sbk in range(nsb):
                        w = min(512, n - sbk * 512)
                        for hi in range(8):
                            r0 = (hi % 2) * 64
                            bk = nbank()
                            mm(ps[:, bk, 0:w], q_idxT[r0:r0 + 64, hi // 2, qt * 128:(qt + 1) * 128],
                               k_idxT[r0:r0 + 64, sbk * 512:sbk * 512 + w], True, True,
                               [("q_idxT",), ("k_idxT",)], [("ps", bk)])
                            rb = rcount % 4
                            rcount += 1
                            act(rbuf[rb][:, 0:w], ps[:, bk, 0:w], AF.Relu, [("ps", bk)], [("rbuf", rb)])
                            if hi == 0:
                                ts("dve", a[:, sbk * 512:sbk * 512 + w], rbuf[rb][:, 0:w], widx_sb[:, qt, 0:1], None, ALU.mult, None,
                                   [("rbuf", rb), "widx_sb"], [atok])
                            else:
                                stt("dve", a[:, sbk * 512:sbk * 512 + w], rbuf[rb][:, 0:w], widx_sb[:, qt, hi:hi + 1],
                                    a[:, sbk * 512:sbk * 512 + w], ALU.mult, ALU.add,
                                    [("rbuf", rb), "widx_sb", atok], [atok])
                    if qt >= 2:
                        P.add("dve", lambda h, a=a, qt=qt, jq=jq: h.tensor_reduce(out=scf(F_LO, jq, jq + 1), in_=a[:, 0:qt * 128],
                                                                                  axis=AX.X, op=ALU.min), [atok], ["sc_lo"])
                        P.add("dve", lambda h, a=a, n=n, jq=jq: h.tensor_reduce(out=scf(F_MX, jq, jq + 1), in_=a[:, 0:n],
                                                                                axis=AX.X, op=ALU.max), [atok], ["sc_mx"])
                    tt("dve", a[:, qt * 128:(qt + 1) * 128], a[:, qt * 128:(qt + 1) * 128], caus_add[:], ALU.add,
                       [atok, "caus_add", "sc_mx"], [atok])
                c0 = 2 if QB == 0 else 0
                tt("dve", scf(F_W, c0), scf(F_MX, c0), scf(F_LO, c0), ALU.subtract, ["sc_lo", "sc_mx"], ["sc_w"])
                for it in range(NIT):
                    f = 2.0 ** -(it + 1)
                    ts("dve", scf(F_HW, c0), scf(F_W, c0), f, None, ALU.mult, None, ["sc_w"], ["sc_hw"])
                    tt("dve", scf(F_MID, c0), scf(F_LO, c0), scf(F_HW, c0), ALU.add, ["sc_lo", "sc_hw"], ["sc_mid"])
                    for jq in range(c0, 4):
                        n = (4 * QB + jq + 1) * 128
                        ts("dve", junk[:, 0:n], acc[jq][:, 0:n], scf(F_MID, jq, jq + 1), None, ALU.is_ge, ALU.add,
                           [("acc", jq), "sc_mid"], ["junk", "sc_cnt"], accum=scf(F_CNT, jq, jq + 1))
                    stt("dve", scf(F_STEP, c0), scf(F_CNT, c0), 255.5, scf(F_HW, c0), ALU.is_gt, ALU.mult,
                        ["sc_cnt", "sc_hw"], ["sc_step"])
                    tt("dve", scf(F_LO, c0), scf(F_LO, c0), scf(F_STEP, c0), ALU.add, ["sc_lo", "sc_step"], ["sc_lo"])
                for jq in range(4):
                    qt = 4 * QB + jq
                    n = (qt + 1) * 128
                    if qt < 2:
                        lo_ap, lo_tok = cst[:, 2:3], "cst"
                    else:
                        lo_ap, lo_tok = scf(F_LO, jq, jq + 1), "sc_lo"
                    mq = mask_qs[jq % 2]
                    mtok = ("mask_q", jq % 2)
                    ts("dve", mq[:, 0:n], acc[jq][:, 0:n], lo_ap, None, ALU.is_ge, None, [("acc", jq), lo_tok], [mtok])
                    for k0 in range(0, qt + 1, 4):
                        nk = min(4, qt + 1 - k0)
                        bk = 4 + tbank[0] % 2
                        tbank[0] += 1
                        for i in range(nk):
                            tr(psb(bk)[:, i * 128:(i + 1) * 128], mq[:, (k0 + i) * 128:(k0 + i + 1) * 128], identb[:],
                               [mtok, "identb"], [("ps", bk)])
                        act(maskT[:, MOFF[QB] + k0:MOFF[QB] + k0 + nk, jq * 128:(jq + 1) * 128],
                            psb(bk)[:, 0:nk * 128].rearrange("p (a b) -> p a b", a=nk), AF.Copy,
                            [("ps", bk)], [("maskT", QB)])
            emit_phase()

            BO = 144 * K
            KA = [view(BO + i * 4 * K, BF16, [L]) for i in range(4)]
            Vg = view(BO + 16 * K, BF16, [16, 4, 65])
            QA = [[view(BO + 26 * K + (b * 4 + i) * K, BF16, [512]) for i in range(4)] for b in range(2)]
            ptb = [view(BO + 34 * K + i * 2 * K, BF16, [1024]) for i in range(4)]
            otok = [view(BO + 42 * K + i * 2 * K, BF16, [4, 256]) for i in range(2)]
            for i in range(4):
                if i % 2 == 1:
                    memset("pool", KA[i][0:64, :], 0.0, [("KA", i)])
                    dma("sp", KA[i][32:40, :], posrows_d, (), [("KA", i)])
                    for b in range(2):
                        memset("pool", QA[b][i][0:64, :], 0.0, [("QA", b, i)])
                else:
                    dma("sp", KA[i][64:72, :], posrows_d, (), [("KA", i)])
            memset("pool", Vg[:, :, :, 64:65], 1.0, ["Vg"])
            hcount = 0
            scount = 0
            for g in range(4):
                for pr in range(2):
                    ptile = 2 * g + pr
                    for tb in range(NB):
                        bk = 4 + (pr * 4 + tb) % 2
                        for cc in range(2):
                            mm(ps[:, bk, :], wuk[:, cc, ptile * 128:(ptile + 1) * 128], c_kvT[:, cc, cols(tb)], cc == 0, cc == 1,
                               ["wuk", ("c_kvT", tb)], [("ps", bk)])
                        act(KA[2 * pr][0:64, cols(tb)], ps[0:64, bk, :], AF.Copy, [("ps", bk)], [("KA", 2 * pr)])
                        cp("dve", KA[2 * pr + 1][64:128, cols(tb)], ps[64:128, bk, :], [("ps", bk)], [("KA", 2 * pr + 1)])
                for s_t in range(NT):
                    bk = 4 + s_t % 2
                    for cc in range(2):
                        mm(ps[:, bk, 0:256], c_kvT[:, cc, s_t * 128:(s_t + 1) * 128], wuv[:, cc, g * 256:(g + 1) * 256],
                           cc == 0, cc == 1, ["wuv", ("c_kvT", s_t // 4)], [("ps", bk)])
                    src = ps[:, bk, 0:256].rearrange("p (a b) -> p a b", a=4)
                    if s_t % 2 == 0:
                        act(Vg[:, s_t, :, 0:64], src, AF.Copy, [("ps", bk)], ["Vg"])
                    else:
                        cp("dve", Vg[:, s_t, :, 0:64], src, [("ps", bk)], ["Vg"])
                items = []
                for QB in range(NB):
                    for hl in range(4):
                        for kt in range(0, 4 * QB, 2):
                            items.append((QB, hl, (kt, kt + 1)))
                        for kt in range(4 * QB, 4 * QB + 4):
                            items.append((QB, hl, (kt,)))
                LA = 2
                obank_of = {}
                pb_of = {}

                def front(i):
                    nonlocal scount
                    QB, hl, kts = items[i]
                    qb = (g * 4 + QB) % 2
                    if hl == 0 and kts[0] == 0:
                        for h2 in range(4):
                            h_abs = 4 * g + h2
                            ptile = 2 * g + h2 // 2
                            if h2 % 2 == 0:
                                cp("pool", QA[qb][h2][0:64, :], Qpair[0:64, ptile, cols(QB)],
                                   [("Qpair", ptile, QB)], [("QA", qb, h2)])
                                dma("sp", QA[qb][h2][64:72, :], qcoef_d[h_abs, :, cols(QB)], (), [("QA", qb, h2)])
                            else:
                                cp("pool", QA[qb][h2][64:128, :], Qpair[64:128, ptile, cols(QB)],
                                   [("Qpair", ptile, QB)], [("QA", qb, h2)])
                                dma("sp", QA[qb][h2][32:40, :], qcoef_d[h_abs, :, cols(QB)], (), [("QA", qb, h2)])
                    kr = 72 if hl % 2 == 0 else 128
                    sl = 2 * (scount % 3)
                    scount += 1
                    pb = i % 4
                    pb_of[i] = pb
                    if len(kts) == 2:
                        for u, kt in enumerate(kts):
                            mm(ps[:, sl + u, :], KA[hl][0:kr, kt * 128:(kt + 1) * 128], QA[qb][hl][0:kr, :],
                               True, True, [("KA", hl), ("QA", qb, hl)], [("ps", sl + u)])
                        pv = ptb[pb].rearrange("p (a b) -> p a b", a=2)
                        act(pv, ps[:, sl:sl + 2, :], AF.Exp, [("ps", sl), ("ps", sl + 1)], [("ptb", pb)])
                        tt("dve", pv, pv, maskT[:, MOFF[QB] + kts[0]:MOFF[QB] + kts[0] + 2, :], ALU.mult,
                           [("ptb", pb), ("maskT", QB)], [("ptb", pb)])
                    else:
                        kt = kts[0]
                        j0 = kt - 4 * QB
                        mm(ps[:, sl, j0 * 128:512], KA[hl][0:kr, kt * 128:(kt + 1) * 128], QA[qb][hl][0:kr, j0 * 128:512],
                           True, False, [("KA", hl), ("QA", qb, hl)], [("ps", sl)])
                        mm(ps[:, sl, j0 * 128:(j0 + 1) * 128], identb[:], causT[:], False, True,
                           ["identb", "causT"], [("ps", sl)])
                        act(ptb[pb][:, j0 * 128:512], ps[:, sl, j0 * 128:512], AF.Exp, [("ps", sl)], [("ptb", pb)])
                        tt("dve", ptb[pb][:, j0 * 128:512], ptb[pb][:, j0 * 128:512],
                           maskT[:, MOFF[QB] + kt, j0 * 128:512], ALU.mult, [("ptb", pb), ("maskT", QB)], [("ptb", pb)])

                def back(i):
                    nonlocal hcount
                    QB, hl, kts = items[i]
                    ob = (g * 4 + QB) % 2
                    if kts[0] == 0:
                        obank_of[(QB, hl)] = 6 + hcount % 2
                        hcount += 1
                    obank = obank_of[(QB, hl)]
                    psO = ps[:, obank, :].rearrange("p (j f) -> p j f", j=4)
                    pb = pb_of[i]
                    for u, kt in enumerate(kts):
                        j0 = max(0, kt - 4 * QB)
                        for j in range(j0, 4):
                            mm(psO[:, j, 0:65], ptb[pb][:, u * 512 + j * 128:u * 512 + (j + 1) * 128], Vg[:, kt, hl, :],
                               kt == 0 and j == 0, kt == 4 * QB + j, [("ptb", pb), "Vg"], [("ps", obank)], skip=True)
                    kt = kts[-1]
                    if kt == 4 * QB + 3:
                        ri = obank - 6
                        rd = rden[:, ri, :]
                        ts("dve", rd, psO[:, :, 64], 1e-30, None, ALU.add, None, [("ps", obank)], [("rden", ri)])
                        P.add("dve", lambda h, rd=rd: h.reciprocal(out=rd, in_=rd), [("rden", ri)], [("rden", ri)])
                        for j in range(4):
                            ts("dve", otok[ob][:, j, hl * 64:(hl + 1) * 64], psO[:, j, 0:64], rden[:, ri, j:j + 1], None,
                               ALU.mult, None, [("ps", obank), ("rden", ri)], [("otok", ob)])
                        if hl == 3:
                            for cc in range(2):
                                bk = 4 + cc
                                for j in range(4):
                                    tr(psb(bk)[:, j * 128:(j + 1) * 128], otok[ob][:, j, cc * 128:(cc + 1) * 128], identb[:],
                                       [("otok", ob), "identb"], [("ps", bk)])
                                act(oT[:, 2 * g + cc, cols(QB)], psb(bk)[:, 0:512], AF.Copy, [("ps", bk)], [("oT", QB)])

                for step in range(len(items) + LA):
                    if step < len(items):
                        front(step)
                    if step - LA >= 0:
                        back(step - LA)
            emit_phase()

            hT = view(96 * K, F32, [8, L])
            hTb = view(0, BF16, [8, L])
            SO = 160 * K
            wst2 = [view(SO + i * 2 * K, BF16, [8, 128]) for i in range(2)]
            xs2 = [view(SO + 4 * K + i * 4 * K, F32, [D]) for i in range(2)]
            zb = [view(SO + 12 * K + i * K, BF16, [512]) for i in range(2)]
            zsq = [view(SO + 14 * K + i * K, BF16, [512]) for i in range(2)]
            mean_t = view(SO + 16 * K, F32, [512])
            msq_t = view(SO + 18 * K, F32, [512])
            rstd_t = view(SO + 20 * K, F32, [512])
            tbuf = [view(SO + 22 * K + i * 2 * K, F32, [512]) for i in range(2)]
            sgb = [view(SO + 26 * K + i * 2 * K, F32, [512]) for i in range(2)]
            actT = view(32 * K, BF16, [NJ, 1024])
            wdn = [view(76 * K + i * 6 * K, BF16, [NJ, 128]) for i in range(2)]
            wgu = [view(88 * K + i * 2 * K, BF16, [8, 128]) for i in range(4)]
            gst = [view(SO + i * 4 * K, F32, [8, 128]) for i in range(3)]
            dst_ = view(SO, F32, [NJ, 128])
            gcount = [0]
            lcount = [0]

            def layer_norm(tb, lyr, which, final_bf16=True):
                gi, bi = (0, 1) if which == 0 else (2, 3)
                for c in range(8):
                    k = lcount[0] % 2
                    lcount[0] += 1
                    cp("dve", zb[k], hT[:, c, cols(tb)], [("hT", c, tb)], [("zb", k)])
                    act(zsq[k], hT[:, c, cols(tb)], AF.Square, [("hT", c, tb)], [("zsq", k)])
                    mm(ps[:, 6, :], onesb[:], zb[k], c == 0, c == 7, ["onesb", ("zb", k)], [("ps", 6)])
                    mm(ps[:, 7, :], onesb[:], zsq[k], c == 0, c == 7, ["onesb", ("zsq", k)], [("ps", 7)])
                ts("dve", mean_t, ps[:, 6, :], 1.0 / D, None, ALU.mult, None, [("ps", 6)], ["mean_t"])
                tt("dve", msq_t, mean_t, mean_t, ALU.mult, ["mean_t"], ["msq_t"])
                stt("dve", msq_t, ps[:, 7, :], 1.0 / D, msq_t, ALU.mult, ALU.subtract, [("ps", 7), "msq_t"], ["msq_t"])
                act(rstd_t, msq_t, AF.Sqrt, ["msq_t", "cst"], ["rstd_t"], bias=cst[:, 1:2], scale=1.0)
                P.add("dve", lambda h: h.reciprocal(out=rstd_t, in_=rstd_t), ["rstd_t"], ["rstd_t"])
                for c in range(8):
                    k = lcount[0] % 2
                    lcount[0] += 1
                    tt("dve", tbuf[k], hT[:, c, cols(tb)], mean_t, ALU.subtract, [("hT", c, tb), "mean_t"], [("tbuf", k)])
                    tt("dve", tbuf[k], tbuf[k], rstd_t, ALU.mult, [("tbuf", k), "rstd_t"], [("tbuf", k)])
                    act(hT[:, c, cols(tb)], tbuf[k], AF.Identity, [("tbuf", k), "lnp"], [("hT", c, tb)],
                        bias=lnp[:, lyr, bi, c:c + 1], scale=lnp[:, lyr, gi, c:c + 1])
                    if final_bf16:
                        ts("pool", hTb[:, c, cols(tb)], tbuf[k], lnp[:, lyr, gi, c:c + 1], lnp[:, lyr, bi, c:c + 1],
                           ALU.mult, ALU.add, [("tbuf", k), "lnp"], [("hTb", c, tb)])

            for tti in range(NT):
                b = tti % 2
                dma("sp", xs2[b], x_d[tti * 128:(tti + 1) * 128, :], (), [("xs2", b)])
                for c in range(8):
                    tr(ps[:, 2 * b + c // 4, (c % 4) * 128:(c % 4 + 1) * 128], xs2[b][:, c * 128:(c + 1) * 128], ident[:],
                       [("xs2", b), "ident"], [("ps", 2 * b + c // 4)])
                act(hT[:, :, tti * 128:(tti + 1) * 128],
                    ps[:, 2 * b:2 * b + 2, :].rearrange("p a (c t) -> p (a c) t", t=128), AF.Copy,
                    [("ps", 2 * b), ("ps", 2 * b + 1)], [("hT", c, tti // 4) for c in range(8)], scale=ALPHA)

            def out_proj(w_d, src, srctok, wst2=wst2, stg=None, alpha=None):
                for it in range(8):
                    wb = it % 2
                    if stg is None:
                        wload(wst2[wb], w_d[it], ("wst3", wb), xs2[wb].rearrange("p (a b) -> p a b", a=8), [("xs2", wb)])
                    else:
                        wload(wst2[wb], w_d[it], ("wst3", wb), stg, ["stgE"])
                    for tb in range(NB):
                        bk = nbank(0, 4)
                        for c in range(8):
                            mm(ps[:, bk, :], wst2[wb][:, c, :], src[:, c, cols(tb)], c == 0, c == 7,
                               [("wst3", wb), (srctok, tb)], [("ps", bk)])
                        if alpha is None:
                            tt("dve", hT[:, it, cols(tb)], hT[:, it, cols(tb)], ps[:, bk, :], ALU.add,
                               [("ps", bk), ("hT", it, tb)], [("hT", it, tb)])
                        else:
                            stt("dve", hT[:, it, cols(tb)], hT[:, it, cols(tb)], alpha, ps[:, bk, :], ALU.mult, ALU.add,
                                [("ps", bk), ("hT", it, tb)], [("hT", it, tb)])
            out_proj(a_wo_d, oT, "oT")
            for tb in range(NB):
                layer_norm(tb, 0, 0)
            snapshot(hT)
            emit_phase()

            def ffn(lyr, last):
                def load_gu(jt):
                    wb = jt % 2
                    k0 = gcount[0] % 3
                    k1 = (gcount[0] + 1) % 3
                    gcount[0] += 2
                    wload(wgu[wb], w_gu_d[lyr, jt], ("wgu", wb), gst[k0], [("gst", k0)], ceng="act")
                    wload(wgu[2 + wb], w_gu_d[lyr, NJ + jt], ("wgu", 2 + wb), gst[k1], [("gst", k1)], ceng="pool")

                def load_dn(it):
                    wb = it % 2
                    toks = [("gst", 0), ("gst", 1), ("gst", 2)]
                    dma("sp", dst_, w_dn_d[lyr, it], (), toks)
                    act(wdn[wb][:, 0:11, :], dst_[:, 0:11, :], AF.Copy, toks, [("wdn", wb, 0)])
                    cp("pool", wdn[wb][:, 11:NJ, :], dst_[:, 11:NJ, :], toks, [("wdn", wb, 1)])

                for half in range(2):
                    load_gu(0)
                    for jt in range(NJ):
                        wb = jt % 2
                        if jt + 1 < NJ:
                            load_gu(jt + 1)
                        for t2 in range(2):
                            tb = 2 * half + t2
                            bg = nbank(0, 4)
                            bu = nbank(0, 4)
                            for c in range(8):
                                mm(ps[:, bg, :], wgu[wb][:, c, :], hTb[:, c, cols(tb)], c == 0, c == 7,
                                   [("wgu", wb), ("hTb", c, tb)], [("ps", bg)])
                            for c in range(8):
                                mm(ps[:, bu, :], wgu[2 + wb][:, c, :], hTb[:, c, cols(tb)], c == 0, c == 7,
                                   [("wgu", 2 + wb), ("hTb", c, tb)], [("ps", bu)])
                            k = (jt * 2 + t2) % 2
                            act(sgb[k], ps[:, bg, :], AF.Silu, [("ps", bg)], [("sgb", k)])
                            tt("dve", actT[:, jt, t2 * 512:(t2 + 1) * 512], sgb[k], ps[:, bu, :], ALU.mult,
                               [("sgb", k), ("ps", bu)], [("actT", t2)])
                    load_dn(0)
                    for it in range(8):
                        wb = it % 2
                        if it + 1 < 8:
                            load_dn(it + 1)
                        for t2 in range(2):
                            tb = 2 * half + t2
                            bk = nbank(0, 4)
                            for jt in range(NJ):
                                mm(ps[:, bk, :], wdn[wb][:, jt, :], actT[:, jt, t2 * 512:(t2 + 1) * 512], jt == 0, jt == NJ - 1,
                                   [("wdn", wb, 0), ("wdn", wb, 1), ("actT", t2)], [("ps", bk)])
                            stt("dve", hT[:, it, cols(tb)], hT[:, it, cols(tb)], ALPHA, ps[:, bk, :], ALU.mult, ALU.add,
                                [("ps", bk), ("hT", it, tb)], [("hT", it, tb)])
                    for t2 in range(2):
                        layer_norm(2 * half + t2, lyr, 1, final_bf16=not last)
                snapshot(hT)
                emit_phase()
            ffn(0, False)

            pooledT = view(32 * K, BF16, [8, L])
            ysT = view(64 * K, BF16, [8, L])
            UP = (16 + L) * 4
            upad = [view(64 * K + i * UP, F32, [16 + L]) for i in range(2)]
            sA = view(64 * K + 2 * UP, F32, [16 + L])
            sB = view(SO + 2 * UP, F32, [16 + L])
            wst3 = [view(SO + 3 * UP + i * 2 * K, BF16, [8, 128]) for i in range(2)]
            wgr = view(SO + 3 * UP + 4 * K, BF16, [2, 256])
            fix = view(SO + 3 * UP + 5 * K, F32, [16])
            stgE = view(SO + 3 * UP + 6 * K, F32, [8, 128])
            stgE2 = view(SO + 3 * UP + 6 * K, F32, [2, 256])
            for t_ in (upad[0], upad[1], sA, sB):
                memset("pool", t_[:, 0:16], 0.0, ["pads"])
            for mt in range(8):
                gi = mt // 2
                w = WINS[gi]
                wb = mt % 2
                u = upad[wb]
                utok = ("upad", wb)
                wload(wst3[wb], b_win_d[mt], ("wst3", wb), stgE, ["stgE"])
                for tb in range(NB):
                    bk = nbank(0, 4)
                    for c in range(8):
                        mm(ps[:, bk, :], wst3[wb][:, c, :], hTb[:, c, cols(tb)], c == 0, c == 7,
                           [("wst3", wb), ("hTb", c, tb)], [("ps", bk)])
                    act(u[:, 16 + tb * 512:16 + (tb + 1) * 512], ps[:, bk, :], AF.Copy, [("ps", bk), "pads"], [utok])
                tt("dve", sA[:, 16:], u[:, 16:], u[:, 15:15 + L], ALU.add, [utok, "pads"], ["sA"])
                cur, curtok = sA, "sA"
                if w >= 4:
                    tt("dve", sB[:, 16:], sA[:, 16:], sA[:, 14:14 + L], ALU.add, ["sA", "pads"], ["sB"])
                    cur, curtok = sB, "sB"
                if w >= 8:
                    tt("dve", sA[:, 16:], sB[:, 16:], sB[:, 12:12 + L], ALU.add, ["sB", "pads"], ["sA"])
                    cur, curtok = sA, "sA"
                if w >= 16:
                    tt("dve", sB[:, 16:], sA[:, 16:], sA[:, 8:8 + L], ALU.add, ["sA", "pads"], ["sB"])
                    cur, curtok = sB, "sB"
                stt("dve", pooledT[:, mt, :], cur[:, 16:], 1.0 / w, u[:, 16:], ALU.mult, ALU.subtract,
                    [curtok, utok], [("pooledT", mt)])
                tt("dve", fix[:, 0:16], cur[:, 16:32], invc[:, gi, :], ALU.mult, [curtok, "invc"], ["fix"])
                tt("dve", pooledT[:, mt, 0:16], fix[:, 0:16], u[:, 16:32], ALU.subtract, ["fix", utok], [("pooledT", mt)])
            emit_phase()
            for gi in range(4):
                wload(wgr, b_wgrp_d[gi], "wgr", stgE2, ["stgE"])
                for dt_ in range(2):
                    mo = 2 * gi + dt_
                    for tb in range(NB):
                        bk = nbank(0, 4)
                        for cc in range(2):
                            mm(ps[:, bk, :], wgr[:, cc, dt_ * 128:(dt_ + 1) * 128], pooledT[:, 2 * gi + cc, cols(tb)],
                               cc == 0, cc == 1, ["wgr", ("pooledT", 2 * gi + cc)], [("ps", bk)])
                        act(ysT[:, mo, cols(tb)], ps[:, bk, :], AF.Identity, [("ps", bk), "bscale"], [("ysT", tb)],
                            scale=bscale[:, mo:mo + 1])
            out_proj(b_wo_d, ysT, "ysT", wst2=wst3, stg=stgE, alpha=ALPHA)
            for tb in range(NB):
                layer_norm(tb, 1, 0)
            snapshot(hT)
            emit_phase()

            ffn(1, True)

            for tti in range(NT):
                b = tti % 2
                for c in range(8):
                    tr(ps[:, 2 * b + c // 4, (c % 4) * 128:(c % 4 + 1) * 128], hT[:, c, tti * 128:(tti + 1) * 128], ident[:],
                       [("hT", c, tti // 4), "ident"], [("ps", 2 * b + c // 4)])
                src = ps[:, 2 * b:2 * b + 2, :].rearrange("p a f -> p (a f)")
                if b == 0:
                    cp("dve", xs2[b], src, [("ps", 2 * b), ("ps", 2 * b + 1)], [("xs2", b)])
                else:
                    act(xs2[b], src, AF.Copy, [("ps", 2 * b), ("ps", 2 * b + 1)], [("xs2", b)])
                dma("sp", out_d[tti * 128:(tti + 1) * 128, :], xs2[b], [("xs2", b)], [("out", tti)])
            emit_phase()

        except _Stop:
            pass
    return nc


def _alibi_slopes():
    return np.exp2(-8.0 * np.arange(1, 17, dtype=np.float64) / 16.0)


def _host_consts():
    bf = ml_dtypes.bfloat16
    c = {}
    c["ident"] = np.eye(128, dtype=np.float32)
    c["identb"] = np.eye(128, dtype=np.float32).astype(bf)
    s = np.arange(L)
    pos = np.zeros((8, L), np.float32)
    pos[0] = s // 16
    pos[1] = s // 16
    pos[2] = s % 16
    pos[3] = s % 16
    pos[4] = 1.0
    c["posrows"] = pos.astype(bf)
    sl = _alibi_slopes()
    qc = np.zeros((16, 8, L), np.float32)
    for h in range(16):
        hi = np.float32(sl[h]).astype(bf).astype(np.float32)
        lo = np.float32(sl[h] - float(hi)).astype(bf).astype(np.float32)
        qc[h, 0] = 16.0 * hi
        qc[h, 1] = 16.0 * lo
        qc[h, 2] = hi
        qc[h, 3] = lo
        qc[h, 4] = -(sl[h] * s)
    c["qcoef"] = qc.astype(bf)
    qi = np.arange(128)[:, None]
    si = np.arange(128)[None, :]
    c["caus_add"] = np.where(si <= qi, 0.0, -1e30).astype(np.float32)
    c["causT"] = np.where(qi <= si, 0.0, -30000.0).astype(np.float32).astype(bf)
    invc = np.zeros((128, 4, 16), np.float32)
    for gi, w in enumerate(WINS):
        invc[:, gi, :] = 1.0 / np.minimum(w, np.arange(16) + 1)
    c["invc"] = invc
    return c


def _tiles_kc(w, ncols_tile=128):
    kd, n = w.shape
    return np.ascontiguousarray(w.reshape(kd // 128, 128, n // ncols_tile, ncols_tile).transpose(2, 1, 0, 3))


def _vec_pc(v):
    return np.ascontiguousarray(v.reshape(-1, 128).T)


def _prep_weights(a_w_in, a_w_uk, a_w_uv, a_kv_norm_g, a_w_o, b_w_in, b_w_grp, b_scale, b_w_o,
                  f_w_gu, f_w_down, ln_mix_g, ln_mix_b, ln_ffn_g, ln_ffn_b):
    m = {}
    w_in = a_w_in[0]
    colsel = np.concatenate([np.arange(0, 1792), np.arange(1792, 1856), np.arange(1792, 1856)])
    m["w_in_t"] = _tiles_kc(w_in[:, colsel])
    m["w_widx"] = np.ascontiguousarray(w_in[:, 1856:1864].reshape(8, 128, 8).transpose(1, 0, 2))
    m["w_uk_t"] = np.ascontiguousarray(a_w_uk[0].transpose(1, 0, 2).reshape(2, 128, 1024).transpose(1, 0, 2))
    m["w_uv_t"] = np.ascontiguousarray(a_w_uv[0].transpose(1, 0, 2).reshape(2, 128, 1024).transpose(1, 0, 2))
    m["kvg"] = _vec_pc(a_kv_norm_g[0])
    m["a_wo_t"] = _tiles_kc(a_w_o[0])
    m["b_win_t"] = _tiles_kc(b_w_in[0])
    m["b_wgrp_t"] = np.ascontiguousarray(b_w_grp[0].reshape(4, 2, 128, 256).transpose(0, 2, 1, 3))
    m["b_scale_t"] = _vec_pc(b_scale[0])
    m["b_wo_t"] = _tiles_kc(b_w_o[0])
    m["w_gu_t"] = np.stack([_tiles_kc(f_w_gu[i]) for i in range(2)])
    m["w_dn_t"] = np.stack([_tiles_kc(f_w_down[i]) for i in range(2)])
    lnp = np.zeros((128, 2, 4, 8), np.float32)
    for i in range(2):
        lnp[:, i, 0] = _vec_pc(ln_mix_g[i])
        lnp[:, i, 1] = _vec_pc(ln_mix_b[i])
        lnp[:, i, 2] = _vec_pc(ln_ffn_g[i])
        lnp[:, i, 3] = _vec_pc(ln_ffn_b[i])
    m["lnp"] = lnp
    return {k: np.ascontiguousarray(v, dtype=np.float32) for k, v in m.items()}


_NC_CACHE = {}


def kernel(x, a_w_in, a_w_uk, a_w_uv, a_kv_norm_g, a_w_o, b_w_in, b_w_grp, b_scale, b_w_o,
           f_w_gu, f_w_down, ln_mix_g, ln_mix_b, ln_ffn_g, ln_ffn_b, _debug=False, _stop=99, _ncores=8):
    f = lambda a: np.asarray(a, dtype=np.float32)
    wm = _prep_weights(f(a_w_in), f(a_w_uk), f(a_w_uv), f(a_kv_norm_g), f(a_w_o), f(b_w_in), f(b_w_grp),
                       f(b_scale), f(b_w_o), f(f_w_gu), f(f_w_down), f(ln_mix_g), f(ln_mix_b), f(ln_ffn_g), f(ln_ffn_b))
    wm.update(_host_consts())
    x = f(x)
    ncores = _ncores
    key = (_debug, _stop)
    if key not in _NC_CACHE:
        _NC_CACHE[key] = build_nc(debug=_debug, stop=_stop)
    nc = _NC_CACHE[key]
    in_maps = []
    for i in range(ncores):
        d = dict(wm)
        d["x"] = np.ascontiguousarray(x[i])
        in_maps.append(d)
    res = run_bass_kernel_spmd(nc, in_maps, core_ids=list(range(ncores)))
    out = np.stack([np.asarray(r["out"], dtype=np.float32) for r in res.results], axis=0)
    if _debug:
        return out, [r["dbg"] for r in res.results]
    return out
```

```python
import contextlib
import numpy as np
import ml_dtypes
import concourse.bass as bass
import concourse.mybir as mybir
from concourse.bass_utils import run_bass_kernel_spmd

F32 = mybir.dt.float32
BF16 = mybir.dt.bfloat16
ALU = mybir.AluOpType
AF = mybir.ActivationFunctionType
AX = mybir.AxisListType

L = 2048
D = 1024
NT = 16
NB = 4
NC8 = 8
DFF = 2816
NJ = 22
ALPHA = 4.0 ** 0.25
WINS = (2, 4, 8, 16)
NIT = 12
DEBUG = False
USE_SWDGE = False


class Op:
    __slots__ = ("eng", "fn", "deps", "ddeps", "idx", "dma", "dsem", "dval", "inc", "cnt", "waits", "dwaits")

    def __init__(self, eng, fn, deps, ddeps, idx, dma):
        self.eng, self.fn, self.deps, self.ddeps, self.idx, self.dma = eng, fn, deps, ddeps, idx, dma
        self.dsem = None
        self.dval = 0
        self.inc = False
        self.cnt = 0
        self.waits = []
        self.dwaits = []


class Prog:
    ENGS = ("pe", "act", "dve", "pool", "sp")
    RING = 8

    def __init__(self, nc, stack):
        self.nc = nc
        self.sems = {e: stack.enter_context(nc.semaphore("s_" + e)) for e in self.ENGS}
        self.dsems = {e: [stack.enter_context(nc.semaphore("d_%s%d" % (e, i))) for i in range(self.RING)]
                      for e in ("sp", "pool")}
        self.ndma = {e: 0 for e in ("sp", "pool")}
        self.count = {e: 0 for e in self.ENGS}
        self.reset_phase()

    def reset_phase(self):
        self.ops = {e: [] for e in self.ENGS}
        self.last_w = {}
        self.readers = {}
        self.phase_dma = {}

    def add(self, eng, fn, reads=(), writes=(), dma=False):
        writes = list(writes) + [t for t in reads if isinstance(t, tuple) and t[0] == "ps" and t not in writes]
        deps, ddeps = {}, {}

        def dep(ev):
            if ev[0] == "d":
                k = (ev[1], ev[2])
                if ddeps.get(k, 0) < ev[3]:
                    ddeps[k] = ev[3]
            else:
                if deps.get(ev[0], -1) < ev[1]:
                    deps[ev[0]] = ev[1]
        for t in reads:
            if t in self.last_w:
                dep(self.last_w[t])
        for t in writes:
            if t in self.last_w:
                dep(self.last_w[t])
            for r in self.readers.get(t, ()):
                dep(r)
        idx = len(self.ops[eng])
        op = Op(eng, fn, deps, ddeps, idx, dma)
        if dma:
            n = self.ndma[eng]
            self.ndma[eng] = n + 1
            slot = n % self.RING
            op.dsem = (eng, slot)
            op.dval = 16 * (n // self.RING + 1)
            if n >= self.RING:
                k = (eng, slot)
                if ddeps.get(k, 0) < op.dval - 16:
                    ddeps[k] = op.dval - 16
            ev = ("d", eng, slot, op.dval)
            self.phase_dma[(eng, slot)] = op.dval
        else:
            ev = (eng, idx)
        self.ops[eng].append(op)
        for t in reads:
            self.readers.setdefault(t, []).append(ev)
        for t in writes:
            self.last_w[t] = ev
            self.readers[t] = []
        return op

    def emit(self):
        nc = self.nc
        last = {}
        for e in self.ENGS:
            for op in reversed(self.ops[e]):
                if not op.dma:
                    last[e] = op.idx
                    break
        for e in self.ENGS:
            deps = {e2: i for e2, i in last.items() if not (e2 == e and e in ("pe", "sp"))}
            self.ops[e].append(Op(e, lambda h: h.nop(), deps, dict(self.phase_dma), len(self.ops[e]), False))
        for e in self.ENGS:
            waited, dwaited = {}, {}
            for op in self.ops[e]:
                for se, si in op.deps.items():
                    if se == "pe" and e == "pe":
                        continue
                    if waited.get(se, -1) < si:
                        waited[se] = si
                        op.waits.append((se, si))
                        self.ops[se][si].inc = True
                for k, v in op.ddeps.items():
                    if dwaited.get(k, 0) < v:
                        dwaited[k] = v
                        op.dwaits.append((k, v))
        for e in self.ENGS:
            c = self.count[e]
            for op in self.ops[e]:
                if op.inc:
                    c += 1
                op.cnt = c
            self.count[e] = c
        ops, sems, dsems = self.ops, self.sems, self.dsems

        def run(e, h):
            for op in ops[e]:
                for se, si in op.waits:
                    h.wait_ge(sems[se], ops[se][si].cnt)
                for (q, slot), v in op.dwaits:
                    h.wait_ge(dsems[q][slot], v)
                ins = op.fn(h)
                if op.dma:
                    ins.then_inc(dsems[op.dsem[0]][op.dsem[1]], 16)
                elif op.inc:
                    ins.then_inc(sems[e], 1)
        with nc.Block() as block:
            @block.tensor
            def _(h):
                run("pe", h)

            @block.scalar
            def _(h):
                run("act", h)

            @block.vector
            def _(h):
                run("dve", h)

            @block.gpsimd
            def _(h):
                run("pool", h)

            @block.sync
            def _(h):
                run("sp", h)
        self.reset_phase()


def build_nc(debug=False, stop=99):
    hcount = 0
    nc = bass.Bass("TRN2", target_bir_lowering=False)

    def din(name, shape, dt=F32):
        return nc.dram_tensor(name, list(shape), dt, kind="ExternalInput").ap()
    x_d = din("x", [L, D])
    w_in_d = din("w_in_t", [15, 128, 8, 128])
    w_widx_d = din("w_widx", [128, 8, 8])
    w_uk_d = din("w_uk_t", [128, 2, 1024])
    w_uv_d = din("w_uv_t", [128, 2, 1024])
    kvg_d = din("kvg", [128, 2])
    a_wo_d = din("a_wo_t", [8, 128, 8, 128])
    b_win_d = din("b_win_t", [8, 128, 8, 128])
    b_wgrp_d = din("b_wgrp_t", [4, 128, 2, 256])
    b_scale_d = din("b_scale_t", [128, 8])
    b_wo_d = din("b_wo_t", [8, 128, 8, 128])
    w_gu_d = din("w_gu_t", [2, 44, 128, 8, 128])
    w_dn_d = din("w_dn_t", [2, 8, 128, NJ, 128])
    lnp_d = din("lnp", [128, 2, 4, 8])
    ident_d = din("ident", [128, 128])
    identb_d = din("identb", [128, 128], BF16)
    posrows_d = din("posrows", [8, L], BF16)
    qcoef_d = din("qcoef", [16, 8, L], BF16)
    caus_add_d = din("caus_add", [128, 128])
    causT_d = din("causT", [128, 128], BF16)
    invc_d = din("invc", [128, 4, 16])
    out_d = nc.dram_tensor("out", [L, D], F32, kind="ExternalOutput").ap()
    if debug:
        dbg_d = nc.dram_tensor("dbg", [6, 128, 8, L], F32, kind="ExternalOutput").ap()

    with contextlib.ExitStack() as st:
        P = Prog(nc, st)

        def sb(name, shape, dt):
            return st.enter_context(nc.sbuf_tensor(name, list(shape), dt))
        ident = sb("ident_sb", [128, 128], F32)
        identb = sb("identb_sb", [128, 128], BF16)
        onesb = sb("onesb", [128, 128], BF16)
        lnp = sb("lnp_sb", [128, 2, 4, 8], F32)
        kvg = sb("kvg_sb", [128, 2], F32)
        bscale = sb("bscale_sb", [128, 8], F32)
        caus_add = sb("caus_add_sb", [128, 128], F32)
        causT = sb("causT_sb", [128, 128], BF16)
        invc = sb("invc_sb", [128, 4, 16], F32)
        widx_w = sb("widx_w", [128, 8, 8], BF16)
        wuk = sb("wuk_sb", [128, 2, 1024], BF16)
        wuv = sb("wuv_sb", [128, 2, 1024], BF16)
        cst = sb("cst", [128, 8], F32)
        widx_sb = sb("widx_sb", [128, 16, 8], F32)
        sc = sb("sc", [128, 32], F32)
        rden = sb("rden", [128, 2, 4], F32)
        ARENA_B = 196 * 1024
        arena = sb("arena", [128, ARENA_B // 2], BF16)
        ps = st.enter_context(nc.psum_tensor("ps", [128, 8, 512], F32))

        def view(off, dt, shape):
            n = int(np.prod(shape))
            nbytes = n * (4 if dt == F32 else 2)
            assert off % 4 == 0 and off + nbytes <= ARENA_B, (off, nbytes)
            ap = arena[:, off // 2:(off + nbytes) // 2]
            if dt == F32:
                ap = ap.bitcast(F32)
            if len(shape) == 2:
                ap = ap.rearrange("p (a b) -> p a b", a=shape[0])
            elif len(shape) == 3:
                ap = ap.rearrange("p (a b c) -> p a b c", a=shape[0], b=shape[1])
            return ap
        K = 1024

        def psb(bank):
            return ps[:, bank, :].bitcast(BF16)

        def mm(out, lhsT, rhs, start, stop, reads, writes, skip=False):
            P.add("pe", lambda h: h.matmul(out, lhsT, rhs, start=start, stop=stop, skip_group_check=skip),
                  reads, writes)

        def tr(out, in_, idn, reads, writes):
            P.add("pe", lambda h: h.transpose(out, in_, idn), reads, writes)

        def act(out, in_, func, reads, writes, bias=None, scale=None):
            kw = {}
            if bias is not None:
                kw["bias"] = bias
            if scale is not None:
                kw["scale"] = scale
            P.add("act", lambda h: h.activation(out=out, in_=in_, func=func, **kw), reads, writes)

        def tt(eng, out, in0, in1, op, reads, writes):
            P.add(eng, lambda h: h.tensor_tensor(out=out, in0=in0, in1=in1, op=op), reads, writes)

        def ts(eng, out, in0, s1, s2, op0, op1, reads, writes, accum=None):
            if op1 is None:
                P.add(eng, lambda h: h.tensor_scalar(out=out, in0=in0, scalar1=s1, scalar2=None, op0=op0),
                      reads, writes)
            elif accum is None:
                P.add(eng, lambda h: h.tensor_scalar(out=out, in0=in0, scalar1=s1, scalar2=s2, op0=op0, op1=op1),
                      reads, writes)
            else:
                P.add(eng, lambda h: h.tensor_scalar(out=out, in0=in0, scalar1=s1, scalar2=s2, op0=op0, op1=op1,
                                                     accum_out=accum), reads, writes)

        def stt(eng, out, in0, scalar, in1, op0, op1, reads, writes):
            P.add(eng, lambda h: h.scalar_tensor_tensor(out=out, in0=in0, scalar=scalar, in1=in1, op0=op0, op1=op1),
                  reads, writes)

        def cp(eng, out, in_, reads, writes):
            P.add(eng, lambda h: h.tensor_copy(out=out, in_=in_), reads, writes)

        def dma(q, out, in_, reads, writes):
            if q == "pool":
                P.add(q, lambda h: h.dma_start(out=out, in_=in_, max_dma_last_dim=4096), reads, writes, dma=True)
            else:
                P.add(q, lambda h: h.dma_start(out=out, in_=in_), reads, writes, dma=True)

        def wload(dst, src, dst_tok, stage, stage_toks, ceng="pool"):
            if USE_SWDGE:
                dma("pool", dst, src, (), [dst_tok])
            else:
                dma("sp", stage, src, (), list(stage_toks))
                if ceng == "act":
                    act(dst, stage, AF.Copy, list(stage_toks), [dst_tok])
                else:
                    cp("pool", dst, stage, list(stage_toks), [dst_tok])

        def memset(eng, ap, val, writes):
            P.add(eng, lambda h: h.memset(ap, val), (), writes)

        def cols(tb):
            return slice(tb * 512, (tb + 1) * 512)

        dbg_n = [0]
        phase_n = [0]

        class _Stop(Exception):
            pass

        def emit_phase():
            P.emit()
            phase_n[0] += 1
            if phase_n[0] >= stop:
                raise _Stop()

        def snapshot(hT):
            if debug:
                k = dbg_n[0]
                dbg_n[0] += 1
                dma("sp", dbg_d[k], hT, [("hT", c, tb) for c in range(8) for tb in range(4)], [("dbg", k)])

        try:
            dma("sp", ident[:], ident_d, (), ["ident"])
            dma("sp", identb[:], identb_d, (), ["identb"])
            dma("sp", lnp[:], lnp_d, (), ["lnp"])
            dma("sp", kvg[:], kvg_d, (), ["kvg"])
            dma("sp", bscale[:], b_scale_d, (), ["bscale"])
            dma("sp", caus_add[:], caus_add_d, (), ["caus_add"])
            dma("sp", causT[:], causT_d, (), ["causT"])
            dma("sp", invc[:], invc_d, (), ["invc"])
            wload(widx_w[:], w_widx_d, "widx_w", view(72 * 1024, F32, [8, 8]), ["stg_widx"])
            wload(wuk[:], w_uk_d, "wuk", view(80 * 1024, F32, [2, 1024]), ["stg_wuk"])
            wload(wuv[:], w_uv_d, "wuv", view(88 * 1024, F32, [2, 1024]), ["stg_wuv"])
            memset("pool", onesb[:], 1.0, ["onesb"])
            memset("pool", cst[:, 0:1], 1e-6, ["cst"])
            memset("pool", cst[:, 1:2], 1e-5, ["cst"])
            memset("pool", cst[:, 2:3], -1e29, ["cst"])

            hTb_old = view(0, BF16, [8, L])
            Qpair = view(32 * K, BF16, [8, L])
            oT = view(64 * K, BF16, [8, L])
            MOFF = [0, 4, 12, 24]
            maskT = view(96 * K, BF16, [40, 512])
            c_kvT = view(136 * K, BF16, [2, L])
            c_raw = view(144 * K, F32, [2, L])
            csq = view(160 * K, BF16, [2, L])
            k_idxT = view(168 * K, BF16, [L])
            q_idxT = view(172 * K, BF16, [4, L])
            wst = [view(188 * K + i * 2 * K, BF16, [8, 128]) for i in range(2)]
            xs = [view(64 * K + i * 4 * K, F32, [D]) for i in range(2)]
            stgA = [view(96 * K + i * 4 * K, F32, [8, 128]) for i in range(2)]

            for tti in range(NT):
                b = tti % 2
                dma("sp", xs[b], x_d[tti * 128:(tti + 1) * 128, :], (), [("xs", b)])
                for c in range(8):
                    tr(ps[:, 2 * b + c // 4, (c % 4) * 128:(c % 4 + 1) * 128], xs[b][:, c * 128:(c + 1) * 128], ident[:],
                       [("xs", b), "ident"], [("ps", 2 * b + c // 4)])
                act(hTb_old[:, :, tti * 128:(tti + 1) * 128],
                    ps[:, 2 * b:2 * b + 2, :].rearrange("p a (c t) -> p (a c) t", t=128), AF.Copy,
                    [("ps", 2 * b), ("ps", 2 * b + 1)], [("hTbo", tti // 4)])

            bank_rr = [0]

            def nbank(lo=0, n=4):
                b = lo + bank_rr[0] % n
                bank_rr[0] += 1
                return b
            for tix in range(15):
                wb = tix % 2
                wload(wst[wb], w_in_d[tix], ("wst", wb), stgA[wb], [("stgA", wb)])
                for tb in range(NB):
                    bk = nbank()
                    for c in range(8):
                        mm(ps[:, bk, :], wst[wb][:, c, :], hTb_old[:, c, cols(tb)], c == 0, c == 7,
                           [("wst", wb), ("hTbo", tb)], [("ps", bk)])
                    if tix < 8:
                        act(Qpair[:, tix, cols(tb)], ps[:, bk, :], AF.Copy, [("ps", bk)], [("Qpair", tix, tb)], scale=0.125)
                    elif tix < 10:
                        cp("dve", c_raw[:, tix - 8, cols(tb)], ps[:, bk, :], [("ps", bk)], [("c_raw", tb)])
                        act(csq[:, tix - 8, cols(tb)], ps[:, bk, :], AF.Square, [("ps", bk)], [("csq", tb)])
                    elif tix < 14:
                        act(q_idxT[:, tix - 10, cols(tb)], ps[:, bk, :], AF.Copy, [("ps", bk)], [("q_idxT",)])
                    else:
                        cp("dve", k_idxT[:, cols(tb)], ps[:, bk, :], [("ps", bk)], [("k_idxT",)])
            for tti in range(NT):
                for c in range(8):
                    mm(ps[:, 4, tti * 8:(tti + 1) * 8], hTb_old[:, c, tti * 128:(tti + 1) * 128], widx_w[:, c, :],
                       c == 0, c == 7, [("hTbo", tti // 4), "widx_w"], [("ps", 4)], skip=True)
            cp("dve", widx_sb[:], ps[:, 4, 0:128].rearrange("p (a b) -> p a b", a=16), [("ps", 4)], ["widx_sb"])

            sd_a = xs[0][:, 0:512]
            rstd_a = xs[1][:, 0:512]
            for tb in range(NB):
                bk = 5
                for cc in range(2):
                    mm(ps[:, bk, :], onesb[:], csq[:, cc, cols(tb)], cc == 0, cc == 1, ["onesb", ("csq", tb)], [("ps", bk)])
                act(sd_a, ps[:, bk, :], AF.Sqrt, [("ps", bk), "cst"], [("xs", 0)],
                    bias=cst[:, 0:1], scale=1.0 / 256)
                P.add("dve", lambda h: h.reciprocal(out=rstd_a, in_=sd_a), [("xs", 0)], [("xs", 1)])
                for cc in range(2):
                    stt("dve", c_kvT[:, cc, cols(tb)], c_raw[:, cc, cols(tb)], kvg[:, cc:cc + 1], rstd_a, ALU.mult, ALU.mult,
                        [("c_raw", tb), "kvg", ("xs", 1)], [("c_kvT", tb)])
            emit_phase()

            acc = [view(i * 8 * K, F32, [L]) for i in range(4)]
            A3O = 144 * K
            rbuf = [view(A3O + i * 2 * K, F32, [512]) for i in range(4)]
            junk = view(A3O + 8 * K, BF16, [L])
            mask_qs = [view(A3O + 12 * K + i * 4 * K, BF16, [L]) for i in range(2)]
            tbank = [0]
            F_LO, F_MX, F_W, F_HW, F_MID, F_CNT, F_STEP = range(7)

            def scf(f, c0=0, c1=4):
                return sc[:, f * 4 + c0:f * 4 + c1]
            rcount = 0
            for QB in range(NB):
                for jq in range(4):
                    qt = 4 * QB + jq
                    n = (qt + 1) * 128
                    a = acc[jq]
                    atok = ("acc", jq)
                    nsb = (n + 511) // 512
                    for sbk in range(nsb):
                        w = min(512, n - sbk * 512)
                        for hi in range(8):
                            r0 = (hi % 2) * 64
                            bk = nbank()
                            mm(ps[:, bk, 0:w], q_idxT[r0:r0 + 64, hi // 2, qt * 128:(qt + 1) * 128],
                               k_idxT[r0:r0 + 64, sbk * 512:sbk * 512 + w], True, True,
                               [("q_idxT",), ("k_idxT",)], [("ps", bk)])
                            rb = rcount % 4
                            rcount += 1
                            act(rbuf[rb][:, 0:w], ps[:, bk, 0:w], AF.Relu, [("ps", bk)], [("rbuf", rb)])
                            if hi == 0:
                                ts("dve", a[:, sbk * 512:sbk * 512 + w], rbuf[rb][:, 0:w], widx_sb[:, qt, 0:1], None, ALU.mult, None,
                                   [("rbuf", rb), "widx_sb"], [atok])
                            else:
                                stt("dve", a[:, sbk * 512:sbk * 512 + w], rbuf[rb][:, 0:w], widx_sb[:, qt, hi:hi + 1],
                                    a[:, sbk * 512:sbk * 512 + w], ALU.mult, ALU.add,
                                    [("rbuf", rb), "widx_sb", atok], [atok])
                    if qt >= 2:
                        P.add("dve", lambda h, a=a, qt=qt, jq=jq: h.tensor_reduce(out=scf(F_LO, jq, jq + 1), in_=a[:, 0:qt * 128],
                                                                                  axis=AX.X, op=ALU.min), [atok], ["sc_lo"])
                        P.add("dve", lambda h, a=a, n=n, jq=jq: h.tensor_reduce(out=scf(F_MX, jq, jq + 1), in_=a[:, 0:n],
                                                                                axis=AX.X, op=ALU.max), [atok], ["sc_mx"])
                    tt("dve", a[:, qt * 128:(qt + 1) * 128], a[:, qt * 128:(qt + 1) * 128], caus_add[:], ALU.add,
                       [atok, "caus_add", "sc_mx"], [atok])
                c0 = 2 if QB == 0 else 0
                tt("dve", scf(F_W, c0), scf(F_MX, c0), scf(F_LO, c0), ALU.subtract, ["sc_lo", "sc_mx"], ["sc_w"])
                for it in range(NIT):
                    f = 2.0 ** -(it + 1)
                    ts("dve", scf(F_HW, c0), scf(F_W, c0), f, None, ALU.mult, None, ["sc_w"], ["sc_hw"])
                    tt("dve", scf(F_MID, c0), scf(F_LO, c0), scf(F_HW, c0), ALU.add, ["sc_lo", "sc_hw"], ["sc_mid"])
                    for jq in range(c0, 4):
                        n = (4 * QB + jq + 1) * 128
                        ts("dve", junk[:, 0:n], acc[jq][:, 0:n], scf(F_MID, jq, jq + 1), None, ALU.is_ge, ALU.add,
                           [("acc", jq), "sc_mid"], ["junk", "sc_cnt"], accum=scf(F_CNT, jq, jq + 1))
                    stt("dve", scf(F_STEP, c0), scf(F_CNT, c0), 255.5, scf(F_HW, c0), ALU.is_gt, ALU.mult,
                        ["sc_cnt", "sc_hw"], ["sc_step"])
                    tt("dve", scf(F_LO, c0), scf(F_LO, c0), scf(F_STEP, c0), ALU.add, ["sc_lo", "sc_step"], ["sc_lo"])
                for jq in range(4):
                    qt = 4 * QB + jq
                    n = (qt + 1) * 128
                    if qt < 2:
                        lo_ap, lo_tok = cst[:, 2:3], "cst"
                    else:
                        lo_ap, lo_tok = scf(F_LO, jq, jq + 1), "sc_lo"
                    mq = mask_qs[jq % 2]
                    mtok = ("mask_q", jq % 2)
                    ts("dve", mq[:, 0:n], acc[jq][:, 0:n], lo_ap, None, ALU.is_ge, None, [("acc", jq), lo_tok], [mtok])
                    for k0 in range(0, qt + 1, 4):
                        nk = min(4, qt + 1 - k0)
                        bk = 4 + tbank[0] % 2
                        tbank[0] += 1
                        for i in range(nk):
                            tr(psb(bk)[:, i * 128:(i + 1) * 128], mq[:, (k0 + i) * 128:(k0 + i + 1) * 128], identb[:],
                               [mtok, "identb"], [("ps", bk)])
                        act(maskT[:, MOFF[QB] + k0:MOFF[QB] + k0 + nk, jq * 128:(jq + 1) * 128],
                            psb(bk)[:, 0:nk * 128].rearrange("p (a b) -> p a b", a=nk), AF.Copy,
                            [("ps", bk)], [("maskT", QB)])
            emit_phase()

            BO = 144 * K
            KA = [view(BO + i * 4 * K, BF16, [L]) for i in range(4)]
            Vg = view(BO + 16 * K, BF16, [16, 4, 65])
            QA = [[view(BO + 26 * K + (b * 4 + i) * K, BF16, [512]) for i in range(4)] for b in range(2)]
            ptb = [view(BO + 34 * K + i * K, BF16, [512]) for i in range(8)]
            otok = [view(BO + 42 * K + i * 2 * K, BF16, [4, 256]) for i in range(2)]
            for i in range(4):
                if i % 2 == 1:
                    memset("pool", KA[i][0:64, :], 0.0, [("KA", i)])
                    dma("sp", KA[i][32:40, :], posrows_d, (), [("KA", i)])
                    for b in range(2):
                        memset("pool", QA[b][i][0:64, :], 0.0, [("QA", b, i)])
                else:
                    dma("sp", KA[i][64:72, :], posrows_d, (), [("KA", i)])
            memset("pool", Vg[:, :, :, 64:65], 1.0, ["Vg"])
            hcount = 0
            for g in range(4):
                for pr in range(2):
                    ptile = 2 * g + pr
                    for tb in range(NB):
                        bk = 4 + (pr * 4 + tb) % 2
                        for cc in range(2):
                            mm(ps[:, bk, :], wuk[:, cc, ptile * 128:(ptile + 1) * 128], c_kvT[:, cc, cols(tb)], cc == 0, cc == 1,
                               ["wuk", ("c_kvT", tb)], [("ps", bk)])
                        act(KA[2 * pr][0:64, cols(tb)], ps[0:64, bk, :], AF.Copy, [("ps", bk)], [("KA", 2 * pr)])
                        cp("dve", KA[2 * pr + 1][64:128, cols(tb)], ps[64:128, bk, :], [("ps", bk)], [("KA", 2 * pr + 1)])
                for s_t in range(NT):
                    bk = 4 + s_t % 2
                    for cc in range(2):
                        mm(ps[:, bk, 0:256], c_kvT[:, cc, s_t * 128:(s_t + 1) * 128], wuv[:, cc, g * 256:(g + 1) * 256],
                           cc == 0, cc == 1, ["wuv", ("c_kvT", s_t // 4)], [("ps", bk)])
                    src = ps[:, bk, 0:256].rearrange("p (a b) -> p a b", a=4)
                    if s_t % 2 == 0:
                        act(Vg[:, s_t, :, 0:64], src, AF.Copy, [("ps", bk)], ["Vg"])
                    else:
                        cp("dve", Vg[:, s_t, :, 0:64], src, [("ps", bk)], ["Vg"])
                items = [(QB, hl, kt) for QB in range(NB) for hl in range(4) for kt in range(4 * QB + 4)]
                LA = 5
                obank_of = {}
                pb_of = {}

                def front(i):
                    QB, hl, kt = items[i]
                    qb = (g * 4 + QB) % 2
                    if hl == 0 and kt == 0:
                        for h2 in range(4):
                            h_abs = 4 * g + h2
                            ptile = 2 * g + h2 // 2
                            if h2 % 2 == 0:
                                cp("pool", QA[qb][h2][0:64, :], Qpair[0:64, ptile, cols(QB)],
                                   [("Qpair", ptile, QB)], [("QA", qb, h2)])
                                dma("sp", QA[qb][h2][64:72, :], qcoef_d[h_abs, :, cols(QB)], (), [("QA", qb, h2)])
                            else:
                                cp("pool", QA[qb][h2][64:128, :], Qpair[64:128, ptile, cols(QB)],
                                   [("Qpair", ptile, QB)], [("QA", qb, h2)])
                                dma("sp", QA[qb][h2][32:40, :], qcoef_d[h_abs, :, cols(QB)], (), [("QA", qb, h2)])
                    kr = 72 if hl % 2 == 0 else 128
                    j0 = max(0, kt - 4 * QB)
                    sbank = nbank(0, 6)
                    mm(ps[:, sbank, j0 * 128:512], KA[hl][0:kr, kt * 128:(kt + 1) * 128], QA[qb][hl][0:kr, j0 * 128:512],
                       True, kt < 4 * QB, [("KA", hl), ("QA", qb, hl)], [("ps", sbank)])
                    if kt >= 4 * QB:
                        mm(ps[:, sbank, j0 * 128:(j0 + 1) * 128], identb[:], causT[:], False, True,
                           ["identb", "causT"], [("ps", sbank)])
                    pb = i % 8
                    pb_of[i] = pb
                    act(ptb[pb][:, j0 * 128:512], ps[:, sbank, j0 * 128:512], AF.Exp, [("ps", sbank)], [("ptb", pb)])
                    tt("dve", ptb[pb][:, j0 * 128:512], ptb[pb][:, j0 * 128:512],
                       maskT[:, MOFF[QB] + kt, j0 * 128:512], ALU.mult, [("ptb", pb), ("maskT", QB)], [("ptb", pb)])

                def back(i):
                    nonlocal hcount
                    QB, hl, kt = items[i]
                    ob = (g * 4 + QB) % 2
                    if kt == 0:
                        obank_of[(QB, hl)] = 6 + hcount % 2
                        hcount += 1
                    obank = obank_of[(QB, hl)]
                    psO = ps[:, obank, :].rearrange("p (j f) -> p j f", j=4)
                    j0 = max(0, kt - 4 * QB)
                    pb = pb_of[i]
                    for j in range(j0, 4):
                        mm(psO[:, j, 0:65], ptb[pb][:, j * 128:(j + 1) * 128], Vg[:, kt, hl, :],
                           kt == 0 and j == 0, kt == 4 * QB + j, [("ptb", pb), "Vg"], [("ps", obank)], skip=True)
                    if kt == 4 * QB + 3:
                        ri = obank - 6
                        rd = rden[:, ri, :]
                        ts("dve", rd, psO[:, :, 64], 1e-30, None, ALU.add, None, [("ps", obank)], [("rden", ri)])
                        P.add("dve", lambda h, rd=rd: h.reciprocal(out=rd, in_=rd), [("rden", ri)], [("rden", ri)])
                        for j in range(4):
                            ts("dve", otok[ob][:, j, hl * 64:(hl + 1) * 64], psO[:, j, 0:64], rden[:, ri, j:j + 1], None,
                               ALU.mult, None, [("ps", obank), ("rden", ri)], [("otok", ob)])
                        if hl == 3:
                            for cc in range(2):
                                bk = 4 + cc
                                for j in range(4):
                                    tr(psb(bk)[:, j * 128:(j + 1) * 128], otok[ob][:, j, cc * 128:(cc + 1) * 128], identb[:],
                                       [("otok", ob), "identb"], [("ps", bk)])
                                act(oT[:, 2 * g + cc, cols(QB)], psb(bk)[:, 0:512], AF.Copy, [("ps", bk)], [("oT", QB)])

                for step in range(len(items) + LA):
                    if step < len(items):
                        front(step)
                    if step - LA >= 0:
                        back(step - LA)
            emit_phase()

            hT = view(96 * K, F32, [8, L])
            hTb = view(0, BF16, [8, L])
            SO = 160 * K
            wst2 = [view(SO + i * 2 * K, BF16, [8, 128]) for i in range(2)]
            xs2 = [view(SO + 4 * K + i * 4 * K, F32, [D]) for i in range(2)]
            zb = [view(SO + 12 * K + i * K, BF16, [512]) for i in range(2)]
            zsq = [view(SO + 14 * K + i * K, BF16, [512]) for i in range(2)]
            mean_t = view(SO + 16 * K, F32, [512])
            msq_t = view(SO + 18 * K, F32, [512])
            rstd_t = view(SO + 20 * K, F32, [512])
            tbuf = [view(SO + 22 * K + i * 2 * K, F32, [512]) for i in range(2)]
            sgb = [view(SO + 26 * K + i * 2 * K, F32, [512]) for i in range(2)]
            actT = view(32 * K, BF16, [NJ, 1024])
            wdn = [view(76 * K + i * 6 * K, BF16, [NJ, 128]) for i in range(2)]
            wgu = [view(88 * K + i * 2 * K, BF16, [8, 128]) for i in range(4)]
            gst = [view(SO + i * 4 * K, F32, [8, 128]) for i in range(3)]
            dst_ = view(SO, F32, [NJ, 128])
            gcount = [0]
            lcount = [0]

            def layer_norm_gen(tb, lyr, which, final_bf16=True):
                gi, bi = (0, 1) if which == 0 else (2, 3)
                for c in range(8):
                    k = lcount[0] % 2
                    lcount[0] += 1
                    cp("dve", zb[k], hT[:, c, cols(tb)], [("hT", c, tb)], [("zb", k)])
                    act(zsq[k], hT[:, c, cols(tb)], AF.Square, [("hT", c, tb)], [("zsq", k)])
                    mm(ps[:, 6, :], onesb[:], zb[k], c == 0, c == 7, ["onesb", ("zb", k)], [("ps", 6)])
                    mm(ps[:, 7, :], onesb[:], zsq[k], c == 0, c == 7, ["onesb", ("zsq", k)], [("ps", 7)])
                    yield
                ts("dve", mean_t, ps[:, 6, :], 1.0 / D, None, ALU.mult, None, [("ps", 6)], ["mean_t"])
                tt("dve", msq_t, mean_t, mean_t, ALU.mult, ["mean_t"], ["msq_t"])
                stt("dve", msq_t, ps[:, 7, :], 1.0 / D, msq_t, ALU.mult, ALU.subtract, [("ps", 7), "msq_t"], ["msq_t"])
                act(rstd_t, msq_t, AF.Sqrt, ["msq_t", "cst"], ["rstd_t"], bias=cst[:, 1:2], scale=1.0)
                P.add("dve", lambda h: h.reciprocal(out=rstd_t, in_=rstd_t), ["rstd_t"], ["rstd_t"])
                yield
                for c in range(8):
                    k = lcount[0] % 2
                    lcount[0] += 1
                    tt("dve", tbuf[k], hT[:, c, cols(tb)], mean_t, ALU.subtract, [("hT", c, tb), "mean_t"], [("tbuf", k)])
                    tt("dve", tbuf[k], tbuf[k], rstd_t, ALU.mult, [("tbuf", k), "rstd_t"], [("tbuf", k)])
                    act(hT[:, c, cols(tb)], tbuf[k], AF.Identity, [("tbuf", k), "lnp"], [("hT", c, tb)],
                        bias=lnp[:, lyr, bi, c:c + 1], scale=lnp[:, lyr, gi, c:c + 1])
                    if final_bf16:
                        ts("pool", hTb[:, c, cols(tb)], tbuf[k], lnp[:, lyr, gi, c:c + 1], lnp[:, lyr, bi, c:c + 1],
                           ALU.mult, ALU.add, [("tbuf", k), "lnp"], [("hTb", c, tb)])
                    yield

            def layer_norm(tb, lyr, which, final_bf16=True):
                for _ in layer_norm_gen(tb, lyr, which, final_bf16):
                    pass

            for tti in range(NT):
                b = tti % 2
                dma("sp", xs2[b], x_d[tti * 128:(tti + 1) * 128, :], (), [("xs2", b)])
                for c in range(8):
                    tr(ps[:, 2 * b + c // 4, (c % 4) * 128:(c % 4 + 1) * 128], xs2[b][:, c * 128:(c + 1) * 128], ident[:],
                       [("xs2", b), "ident"], [("ps", 2 * b + c // 4)])
                act(hT[:, :, tti * 128:(tti + 1) * 128],
                    ps[:, 2 * b:2 * b + 2, :].rearrange("p a (c t) -> p (a c) t", t=128), AF.Copy,
                    [("ps", 2 * b), ("ps", 2 * b + 1)], [("hT", c, tti // 4) for c in range(8)], scale=ALPHA)

            def out_proj(w_d, src, srctok, wst2=wst2, stg=None, alpha=None):
                for it in range(8):
                    wb = it % 2
                    if stg is None:
                        wload(wst2[wb], w_d[it], ("wst3", wb), xs2[wb].rearrange("p (a b) -> p a b", a=8), [("xs2", wb)])
                    else:
                        wload(wst2[wb], w_d[it], ("wst3", wb), stg, ["stgE"])
                    for tb in range(NB):
                        bk = nbank(0, 4)
                        for c in range(8):
                            mm(ps[:, bk, :], wst2[wb][:, c, :], src[:, c, cols(tb)], c == 0, c == 7,
                               [("wst3", wb), (srctok, tb)], [("ps", bk)])
                        if alpha is None:
                            tt("dve", hT[:, it, cols(tb)], hT[:, it, cols(tb)], ps[:, bk, :], ALU.add,
                               [("ps", bk), ("hT", it, tb)], [("hT", it, tb)])
                        else:
                            stt("dve", hT[:, it, cols(tb)], hT[:, it, cols(tb)], alpha, ps[:, bk, :], ALU.mult, ALU.add,
                                [("ps", bk), ("hT", it, tb)], [("hT", it, tb)])
            out_proj(a_wo_d, oT, "oT")
            for tb in range(NB):
                layer_norm(tb, 0, 0)
            snapshot(hT)
            emit_phase()

            def ffn(lyr, last):
                def load_gu(jt):
                    wb = jt % 2
                    k0 = gcount[0] % 3
                    k1 = (gcount[0] + 1) % 3
                    gcount[0] += 2
                    wload(wgu[wb], w_gu_d[lyr, jt], ("wgu", wb), gst[k0], [("gst", k0)], ceng="act")
                    wload(wgu[2 + wb], w_gu_d[lyr, NJ + jt], ("wgu", 2 + wb), gst[k1], [("gst", k1)], ceng="pool")

                def load_dn(it):
                    wb = it % 2
                    toks = [("gst", 0), ("gst", 1), ("gst", 2)]
                    dma("sp", dst_, w_dn_d[lyr, it], (), toks)
                    act(wdn[wb][:, 0:11, :], dst_[:, 0:11, :], AF.Copy, toks, [("wdn", wb, 0)])
                    cp("pool", wdn[wb][:, 11:NJ, :], dst_[:, 11:NJ, :], toks, [("wdn", wb, 1)])

                pending = iter(())
                for half in range(2):
                    load_gu(0)
                    for jt in range(NJ):
                        wb = jt % 2
                        if jt + 1 < NJ:
                            load_gu(jt + 1)
                        for _ in range(2):
                            next(pending, None)
                        for t2 in range(2):
                            tb = 2 * half + t2
                            bg = nbank(0, 4)
                            bu = nbank(0, 4)
                            for c in range(8):
                                mm(ps[:, bg, :], wgu[wb][:, c, :], hTb[:, c, cols(tb)], c == 0, c == 7,
                                   [("wgu", wb), ("hTb", c, tb)], [("ps", bg)])
                            for c in range(8):
                                mm(ps[:, bu, :], wgu[2 + wb][:, c, :], hTb[:, c, cols(tb)], c == 0, c == 7,
                                   [("wgu", 2 + wb), ("hTb", c, tb)], [("ps", bu)])
                            k = (jt * 2 + t2) % 2
                            act(sgb[k], ps[:, bg, :], AF.Silu, [("ps", bg)], [("sgb", k)])
                            tt("dve", actT[:, jt, t2 * 512:(t2 + 1) * 512], sgb[k], ps[:, bu, :], ALU.mult,
                               [("sgb", k), ("ps", bu)], [("actT", t2)])
                    load_dn(0)
                    for it in range(8):
                        wb = it % 2
                        if it + 1 < 8:
                            load_dn(it + 1)
                        for t2 in range(2):
                            tb = 2 * half + t2
                            bk = nbank(0, 4)
                            for jt in range(NJ):
                                mm(ps[:, bk, :], wdn[wb][:, jt, :], actT[:, jt, t2 * 512:(t2 + 1) * 512], jt == 0, jt == NJ - 1,
                                   [("wdn", wb, 0), ("wdn", wb, 1), ("actT", t2)], [("ps", bk)])
                            stt("dve", hT[:, it, cols(tb)], hT[:, it, cols(tb)], ALPHA, ps[:, bk, :], ALU.mult, ALU.add,
                                [("ps", bk), ("hT", it, tb)], [("hT", it, tb)])
                    for _ in pending:
                        pass
                    if half == 0:
                        import itertools
                        pending = itertools.chain(layer_norm_gen(0, lyr, 1, not last), layer_norm_gen(1, lyr, 1, not last))
                    else:
                        for t2 in range(2):
                            layer_norm(2 * half + t2, lyr, 1, final_bf16=not last)
                snapshot(hT)
                emit_phase()
            ffn(0, False)

            pooledT = view(32 * K, BF16, [8, L])
            ysT = view(64 * K, BF16, [8, L])
            UP = (16 + L) * 4
            upad = [view(64 * K + i * UP, F32, [16 + L]) for i in range(2)]
            sA = view(64 * K + 2 * UP, F32, [16 + L])
            sB = view(SO + 2 * UP, F32, [16 + L])
            wst3 = [view(SO + 3 * UP + i * 2 * K, BF16, [8, 128]) for i in range(2)]
            wgr = view(SO + 3 * UP + 4 * K, BF16, [2, 256])
            fix = view(SO + 3 * UP + 5 * K, F32, [16])
            stgE = view(SO + 3 * UP + 6 * K, F32, [8, 128])
            stgE2 = view(SO + 3 * UP + 6 * K, F32, [2, 256])
            for t_ in (upad[0], upad[1], sA, sB):
                memset("pool", t_[:, 0:16], 0.0, ["pads"])
            for mt in range(8):
                gi = mt // 2
                w = WINS[gi]
                wb = mt % 2
                u = upad[wb]
                utok = ("upad", wb)
                wload(wst3[wb], b_win_d[mt], ("wst3", wb), stgE, ["stgE"])
                for tb in range(NB):
                    bk = nbank(0, 4)
                    for c in range(8):
                        mm(ps[:, bk, :], wst3[wb][:, c, :], hTb[:, c, cols(tb)], c == 0, c == 7,
                           [("wst3", wb), ("hTb", c, tb)], [("ps", bk)])
                    act(u[:, 16 + tb * 512:16 + (tb + 1) * 512], ps[:, bk, :], AF.Copy, [("ps", bk), "pads"], [utok])
                tt("dve", sA[:, 16:], u[:, 16:], u[:, 15:15 + L], ALU.add, [utok, "pads"], ["sA"])
                cur, curtok = sA, "sA"
                if w >= 4:
                    tt("dve", sB[:, 16:], sA[:, 16:], sA[:, 14:14 + L], ALU.add, ["sA", "pads"], ["sB"])
                    cur, curtok = sB, "sB"
                if w >= 8:
                    tt("dve", sA[:, 16:], sB[:, 16:], sB[:, 12:12 + L], ALU.add, ["sB", "pads"], ["sA"])
                    cur, curtok = sA, "sA"
                if w >= 16:
                    tt("dve", sB[:, 16:], sA[:, 16:], sA[:, 8:8 + L], ALU.add, ["sA", "pads"], ["sB"])
                    cur, curtok = sB, "sB"
                stt("dve", pooledT[:, mt, :], cur[:, 16:], 1.0 / w, u[:, 16:], ALU.mult, ALU.subtract,
                    [curtok, utok], [("pooledT", mt)])
                tt("dve", fix[:, 0:16], cur[:, 16:32], invc[:, gi, :], ALU.mult, [curtok, "invc"], ["fix"])
                tt("dve", pooledT[:, mt, 0:16], fix[:, 0:16], u[:, 16:32], ALU.subtract, ["fix", utok], [("pooledT", mt)])
            emit_phase()
            for gi in range(4):
                wload(wgr, b_wgrp_d[gi], "wgr", stgE2, ["stgE"])
                for dt_ in range(2):
                    mo = 2 * gi + dt_
                    for tb in range(NB):
                        bk = nbank(0, 4)
                        for cc in range(2):
                            mm(ps[:, bk, :], wgr[:, cc, dt_ * 128:(dt_ + 1) * 128], pooledT[:, 2 * gi + cc, cols(tb)],
                               cc == 0, cc == 1, ["wgr", ("pooledT", 2 * gi + cc)], [("ps", bk)])
                        act(ysT[:, mo, cols(tb)], ps[:, bk, :], AF.Identity, [("ps", bk), "bscale"], [("ysT", tb)],
                            scale=bscale[:, mo:mo + 1])
            out_proj(b_wo_d, ysT, "ysT", wst2=wst3, stg=stgE, alpha=ALPHA)
            for tb in range(NB):
                layer_norm(tb, 1, 0)
            snapshot(hT)
            emit_phase()

            ffn(1, True)

            for tti in range(NT):
                b = tti % 2
                for c in range(8):
                    tr(ps[:, 2 * b + c // 4, (c % 4) * 128:(c % 4 + 1) * 128], hT[:, c, tti * 128:(tti + 1) * 128], ident[:],
                       [("hT", c, tti // 4), "ident"], [("ps", 2 * b + c // 4)])
                src = ps[:, 2 * b:2 * b + 2, :].rearrange("p a f -> p (a f)")
                if b == 0:
                    cp("dve", xs2[b], src, [("ps", 2 * b), ("ps", 2 * b + 1)], [("xs2", b)])
                else:
                    act(xs2[b], src, AF.Copy, [("ps", 2 * b), ("ps", 2 * b + 1)], [("xs2", b)])
                dma("sp", out_d[tti * 128:(tti + 1) * 128, :], xs2[b], [("xs2", b)], [("out", tti)])
            emit_phase()

        except _Stop:
            pass
    return nc


def _alibi_slopes():
    return np.exp2(-8.0 * np.arange(1, 17, dtype=np.float64) / 16.0)


def _host_consts():
    bf = ml_dtypes.bfloat16
    c = {}
    c["ident"] = np.eye(128, dtype=np.float32)
    c["identb"] = np.eye(128, dtype=np.float32).astype(bf)
    s = np.arange(L)
    pos = np.zeros((8, L), np.float32)
    pos[0] = s // 16
    pos[1] = s // 16
    pos[2] = s % 16
    pos[3] = s % 16
    pos[4] = 1.0
    c["posrows"] = pos.astype(bf)
    sl = _alibi_slopes()
    qc = np.zeros((16, 8, L), np.float32)
    for h in range(16):
        hi = np.float32(sl[h]).astype(bf).astype(np.float32)
        lo = np.float32(sl[h] - float(hi)).astype(bf).astype(np.float32)
        qc[h, 0] = 16.0 * hi
        qc[h, 1] = 16.0 * lo
        qc[h, 2] = hi
        qc[h, 3] = lo
        qc[h, 4] = -(sl[h] * s)
    c["qcoef"] = qc.astype(bf)
    qi = np.arange(128)[:, None]
    si = np.arange(128)[None, :]
    c["caus_add"] = np.where(si <= qi, 0.0, -1e30).astype(np.float32)
    c["causT"] = np.where(qi <= si, 0.0, -30000.0).astype(np.float32).astype(bf)
    invc = np.zeros((128, 4, 16), np.float32)
    for gi, w in enumerate(WINS):
        invc[:, gi, :] = 1.0 / np.minimum(w, np.arange(16) + 1)
    c["invc"] = invc
    return c


def _tiles_kc(w, ncols_tile=128):
    kd, n = w.shape
    return np.ascontiguousarray(w.reshape(kd // 128, 128, n // ncols_tile, ncols_tile).transpose(2, 1, 0, 3))


def _vec_pc(v):
    return np.ascontiguousarray(v.reshape(-1, 128).T)


def _prep_weights(a_w_in, a_w_uk, a_w_uv, a_kv_norm_g, a_w_o, b_w_in, b_w_grp, b_scale, b_w_o,
                  f_w_gu, f_w_down, ln_mix_g, ln_mix_b, ln_ffn_g, ln_ffn_b):
    m = {}
    w_in = a_w_in[0]
    colsel = np.concatenate([np.arange(0, 1792), np.arange(1792, 1856), np.arange(1792, 1856)])
    m["w_in_t"] = _tiles_kc(w_in[:, colsel])
    m["w_widx"] = np.ascontiguousarray(w_in[:, 1856:1864].reshape(8, 128, 8).transpose(1, 0, 2))
    m["w_uk_t"] = np.ascontiguousarray(a_w_uk[0].transpose(1, 0, 2).reshape(2, 128, 1024).transpose(1, 0, 2))
    m["w_uv_t"] = np.ascontiguousarray(a_w_uv[0].transpose(1, 0, 2).reshape(2, 128, 1024).transpose(1, 0, 2))
    m["kvg"] = _vec_pc(a_kv_norm_g[0])
    m["a_wo_t"] = _tiles_kc(a_w_o[0])
    m["b_win_t"] = _tiles_kc(b_w_in[0])
    m["b_wgrp_t"] = np.ascontiguousarray(b_w_grp[0].reshape(4, 2, 128, 256).transpose(0, 2, 1, 3))
    m["b_scale_t"] = _vec_pc(b_scale[0])
    m["b_wo_t"] = _tiles_kc(b_w_o[0])
    m["w_gu_t"] = np.stack([_tiles_kc(f_w_gu[i]) for i in range(2)])
    m["w_dn_t"] = np.stack([_tiles_kc(f_w_down[i]) for i in range(2)])
    lnp = np.zeros((128, 2, 4, 8), np.float32)
    for i in range(2):
        lnp[:, i, 0] = _vec_pc(ln_mix_g[i])
        lnp[:, i, 1] = _vec_pc(ln_mix_b[i])
        lnp[:, i, 2] = _vec_pc(ln_ffn_g[i])
        lnp[:, i, 3] = _vec_pc(ln_ffn_b[i])
    m["lnp"] = lnp
    return {k: np.ascontiguousarray(v, dtype=np.float32) for k, v in m.items()}


_NC_CACHE = {}


def kernel(x, a_w_in, a_w_uk, a_w_uv, a_kv_norm_g, a_w_o, b_w_in, b_w_grp, b_scale, b_w_o,
           f_w_gu, f_w_down, ln_mix_g, ln_mix_b, ln_ffn_g, ln_ffn_b, _debug=False, _stop=99, _ncores=8):
    f = lambda a: np.asarray(a, dtype=np.float32)
    wm = _prep_weights(f(a_w_in), f(a_w_uk), f(a_w_uv), f(a_kv_norm_g), f(a_w_o), f(b_w_in), f(b_w_grp),
                       f(b_scale), f(b_w_o), f(f_w_gu), f(f_w_down), f(ln_mix_g), f(ln_mix_b), f(ln_ffn_g), f(ln_ffn_b))
    wm.update(_host_consts())
    x = f(x)
    ncores = _ncores
    key = (_debug, _stop)
    if key not in _NC_CACHE:
        _NC_CACHE[key] = build_nc(debug=_debug, stop=_stop)
    nc = _NC_CACHE[key]
    in_maps = []
    for i in range(ncores):
        d = dict(wm)
        d["x"] = np.ascontiguousarray(x[i])
        in_maps.append(d)
    res = run_bass_kernel_spmd(nc, in_maps, core_ids=list(range(ncores)))
    out = np.stack([np.asarray(r["out"], dtype=np.float32) for r in res.results], axis=0)
    if _debug:
        return out, [r["dbg"] for r in res.results]
    return out
```

```python
import contextlib
import numpy as np
import ml_dtypes
import concourse.bass as bass
import concourse.mybir as mybir
from concourse.bass_utils import run_bass_kernel_spmd

F32 = mybir.dt.float32
BF16 = mybir.dt.bfloat16
ALU = mybir.AluOpType
AF = mybir.ActivationFunctionType
AX = mybir.AxisListType

L = 2048
D = 1024
NT = 16
NB = 4
NC8 = 8
DFF = 2816
NJ = 22
ALPHA = 4.0 ** 0.25
WINS = (2, 4, 8, 16)
NIT = 12
DEBUG = False
USE_SWDGE = False


class Op:
    __slots__ = ("eng", "fn", "deps", "ddeps", "idx", "dma", "dsem", "dval", "inc", "cnt", "waits", "dwaits")

    def __init__(self, eng, fn, deps, ddeps, idx, dma):
        self.eng, self.fn, self.deps, self.ddeps, self.idx, self.dma = eng, fn, deps, ddeps, idx, dma
        self.dsem = None
        self.dval = 0
        self.inc = False
        self.cnt = 0
        self.waits = []
        self.dwaits = []


class Prog:
    ENGS = ("pe", "act", "dve", "pool", "sp")
    RING = 8

    def __init__(self, nc, stack):
        self.nc = nc
        self.sems = {e: stack.enter_context(nc.semaphore("s_" + e)) for e in self.ENGS}
        self.dsems = {e: [stack.enter_context(nc.semaphore("d_%s%d" % (e, i))) for i in range(self.RING)]
                      for e in ("sp", "pool")}
        self.ndma = {e: 0 for e in ("sp", "pool")}
        self.count = {e: 0 for e in self.ENGS}
        self.reset_phase()

    def reset_phase(self):
        self.ops = {e: [] for e in self.ENGS}
        self.last_w = {}
        self.readers = {}
        self.phase_dma = {}

    def add(self, eng, fn, reads=(), writes=(), dma=False):
        writes = list(writes) + [t for t in reads if isinstance(t, tuple) and t[0] == "ps" and t not in writes]
        deps, ddeps = {}, {}

        def dep(ev):
            if ev[0] == "d":
                k = (ev[1], ev[2])
                if ddeps.get(k, 0) < ev[3]:
                    ddeps[k] = ev[3]
            else:
                if deps.get(ev[0], -1) < ev[1]:
                    deps[ev[0]] = ev[1]
        for t in reads:
            if t in self.last_w:
                dep(self.last_w[t])
        for t in writes:
            if t in self.last_w:
                dep(self.last_w[t])
            for r in self.readers.get(t, ()):
                dep(r)
        idx = len(self.ops[eng])
        op = Op(eng, fn, deps, ddeps, idx, dma)
        if dma:
            n = self.ndma[eng]
            self.ndma[eng] = n + 1
            slot = n % self.RING
            op.dsem = (eng, slot)
            op.dval = 16 * (n // self.RING + 1)
            if n >= self.RING:
                k = (eng, slot)
                if ddeps.get(k, 0) < op.dval - 16:
                    ddeps[k] = op.dval - 16
            ev = ("d", eng, slot, op.dval)
            self.phase_dma[(eng, slot)] = op.dval
        else:
            ev = (eng, idx)
        self.ops[eng].append(op)
        for t in reads:
            self.readers.setdefault(t, []).append(ev)
        for t in writes:
            self.last_w[t] = ev
            self.readers[t] = []
        return op

    def emit(self):
        nc = self.nc
        last = {}
        for e in self.ENGS:
            for op in reversed(self.ops[e]):
                if not op.dma:
                    last[e] = op.idx
                    break
        for e in self.ENGS:
            deps = {e2: i for e2, i in last.items() if not (e2 == e and e in ("pe", "sp"))}
            self.ops[e].append(Op(e, lambda h: h.nop(), deps, dict(self.phase_dma), len(self.ops[e]), False))
        for e in self.ENGS:
            waited, dwaited = {}, {}
            for op in self.ops[e]:
                for se, si in op.deps.items():
                    if se == "pe" and e == "pe":
                        continue
                    if waited.get(se, -1) < si:
                        waited[se] = si
                        op.waits.append((se, si))
                        self.ops[se][si].inc = True
                for k, v in op.ddeps.items():
                    if dwaited.get(k, 0) < v:
                        dwaited[k] = v
                        op.dwaits.append((k, v))
        for e in self.ENGS:
            c = self.count[e]
            for op in self.ops[e]:
                if op.inc:
                    c += 1
                op.cnt = c
            self.count[e] = c
        ops, sems, dsems = self.ops, self.sems, self.dsems

        def run(e, h):
            for op in ops[e]:
                for se, si in op.waits:
                    h.wait_ge(sems[se], ops[se][si].cnt)
                for (q, slot), v in op.dwaits:
                    h.wait_ge(dsems[q][slot], v)
                ins = op.fn(h)
                if op.dma:
                    ins.then_inc(dsems[op.dsem[0]][op.dsem[1]], 16)
                elif op.inc:
                    ins.then_inc(sems[e], 1)
        with nc.Block() as block:
            @block.tensor
            def _(h):
                run("pe", h)

            @block.scalar
            def _(h):
                run("act", h)

            @block.vector
            def _(h):
                run("dve", h)

            @block.gpsimd
            def _(h):
                run("pool", h)

            @block.sync
            def _(h):
                run("sp", h)
        self.reset_phase()


def build_nc(debug=False, stop=99):
    hcount = 0
    nc = bass.Bass("TRN2", target_bir_lowering=False)

    def din(name, shape, dt=F32):
        return nc.dram_tensor(name, list(shape), dt, kind="ExternalInput").ap()
    x_d = din("x", [L, D])
    w_in_d = din("w_in_t", [15, 128, 8, 128])
    w_widx_d = din("w_widx", [128, 8, 8])
    w_uk_d = din("w_uk_t", [128, 2, 1024])
    w_uv_d = din("w_uv_t", [128, 2, 1024])
    kvg_d = din("kvg", [128, 2])
    a_wo_d = din("a_wo_t", [8, 128, 8, 128])
    b_win_d = din("b_win_t", [8, 128, 8, 128])
    b_wgrp_d = din("b_wgrp_t", [4, 128, 2, 256])
    b_scale_d = din("b_scale_t", [128, 8])
    b_wo_d = din("b_wo_t", [8, 128, 8, 128])
    w_gu_d = din("w_gu_t", [2, 44, 128, 8, 128])
    w_dn_d = din("w_dn_t", [2, 8, 128, NJ, 128])
    lnp_d = din("lnp", [128, 2, 4, 8])
    ident_d = din("ident", [128, 128])
    identb_d = din("identb", [128, 128], BF16)
    posrows_d = din("posrows", [8, L], BF16)
    qcoef_d = din("qcoef", [16, 8, L], BF16)
    caus_add_d = din("caus_add", [128, 128])
    causT_d = din("causT", [128, 128], BF16)
    invc_d = din("invc", [128, 4, 16])
    out_d = nc.dram_tensor("out", [L, D], F32, kind="ExternalOutput").ap()
    if debug:
        dbg_d = nc.dram_tensor("dbg", [6, 128, 8, L], F32, kind="ExternalOutput").ap()

    with contextlib.ExitStack() as st:
        P = Prog(nc, st)

        def sb(name, shape, dt):
            return st.enter_context(nc.sbuf_tensor(name, list(shape), dt))
        ident = sb("ident_sb", [128, 128], F32)
        identb = sb("identb_sb", [128, 128], BF16)
        onesb = sb("onesb", [128, 128], BF16)
        lnp = sb("lnp_sb", [128, 2, 4, 8], F32)
        kvg = sb("kvg_sb", [128, 2], F32)
        bscale = sb("bscale_sb", [128, 8], F32)
        caus_add = sb("caus_add_sb", [128, 128], F32)
        causT = sb("causT_sb", [128, 128], BF16)
        invc = sb("invc_sb", [128, 4, 16], F32)
        widx_w = sb("widx_w", [128, 8, 8], BF16)
        wuk = sb("wuk_sb", [128, 2, 1024], BF16)
        wuv = sb("wuv_sb", [128, 2, 1024], BF16)
        cst = sb("cst", [128, 8], F32)
        widx_sb = sb("widx_sb", [128, 16, 8], F32)
        sc = sb("sc", [128, 36], F32)
        rden = sb("rden", [128, 2, 4], F32)
        ARENA_B = 196 * 1024
        arena = sb("arena", [128, ARENA_B // 2], BF16)
        ps = st.enter_context(nc.psum_tensor("ps", [128, 8, 512], F32))

        def view(off, dt, shape):
            n = int(np.prod(shape))
            nbytes = n * (4 if dt == F32 else 2)
            assert off % 4 == 0 and off + nbytes <= ARENA_B, (off, nbytes)
            ap = arena[:, off // 2:(off + nbytes) // 2]
            if dt == F32:
                ap = ap.bitcast(F32)
            if len(shape) == 2:
                ap = ap.rearrange("p (a b) -> p a b", a=shape[0])
            elif len(shape) == 3:
                ap = ap.rearrange("p (a b c) -> p a b c", a=shape[0], b=shape[1])
            return ap
        K = 1024

        def psb(bank):
            return ps[:, bank, :].bitcast(BF16)

        def mm(out, lhsT, rhs, start, stop, reads, writes, skip=False):
            P.add("pe", lambda h: h.matmul(out, lhsT, rhs, start=start, stop=stop, skip_group_check=skip),
                  reads, writes)

        def tr(out, in_, idn, reads, writes):
            P.add("pe", lambda h: h.transpose(out, in_, idn), reads, writes)

        def act(out, in_, func, reads, writes, bias=None, scale=None):
            kw = {}
            if bias is not None:
                kw["bias"] = bias
            if scale is not None:
                kw["scale"] = scale
            P.add("act", lambda h: h.activation(out=out, in_=in_, func=func, **kw), reads, writes)

        def tt(eng, out, in0, in1, op, reads, writes):
            P.add(eng, lambda h: h.tensor_tensor(out=out, in0=in0, in1=in1, op=op), reads, writes)

        def ts(eng, out, in0, s1, s2, op0, op1, reads, writes, accum=None):
            if op1 is None:
                P.add(eng, lambda h: h.tensor_scalar(out=out, in0=in0, scalar1=s1, scalar2=None, op0=op0),
                      reads, writes)
            elif accum is None:
                P.add(eng, lambda h: h.tensor_scalar(out=out, in0=in0, scalar1=s1, scalar2=s2, op0=op0, op1=op1),
                      reads, writes)
            else:
                P.add(eng, lambda h: h.tensor_scalar(out=out, in0=in0, scalar1=s1, scalar2=s2, op0=op0, op1=op1,
                                                     accum_out=accum), reads, writes)

        def stt(eng, out, in0, scalar, in1, op0, op1, reads, writes):
            P.add(eng, lambda h: h.scalar_tensor_tensor(out=out, in0=in0, scalar=scalar, in1=in1, op0=op0, op1=op1),
                  reads, writes)

        def cp(eng, out, in_, reads, writes):
            P.add(eng, lambda h: h.tensor_copy(out=out, in_=in_), reads, writes)

        def dma(q, out, in_, reads, writes):
            if q == "pool":
                P.add(q, lambda h: h.dma_start(out=out, in_=in_, max_dma_last_dim=4096), reads, writes, dma=True)
            else:
                P.add(q, lambda h: h.dma_start(out=out, in_=in_), reads, writes, dma=True)

        def wload(dst, src, dst_tok, stage, stage_toks, ceng="pool"):
            if USE_SWDGE:
                dma("pool", dst, src, (), [dst_tok])
            else:
                dma("sp", stage, src, (), list(stage_toks))
                if ceng == "act":
                    act(dst, stage, AF.Copy, list(stage_toks), [dst_tok])
                else:
                    cp("pool", dst, stage, list(stage_toks), [dst_tok])

        def memset(eng, ap, val, writes):
            P.add(eng, lambda h: h.memset(ap, val), (), writes)

        def cols(tb):
            return slice(tb * 512, (tb + 1) * 512)

        dbg_n = [0]
        phase_n = [0]

        class _Stop(Exception):
            pass

        def emit_phase():
            P.emit()
            phase_n[0] += 1
            if phase_n[0] >= stop:
                raise _Stop()

        def snapshot(hT):
            if debug:
                k = dbg_n[0]
                dbg_n[0] += 1
                dma("sp", dbg_d[k], hT, [("hT", c, tb) for c in range(8) for tb in range(4)], [("dbg", k)])

        try:
            dma("sp", ident[:], ident_d, (), ["ident"])
            dma("sp", identb[:], identb_d, (), ["identb"])
            dma("sp", lnp[:], lnp_d, (), ["lnp"])
            dma("sp", kvg[:], kvg_d, (), ["kvg"])
            dma("sp", bscale[:], b_scale_d, (), ["bscale"])
            dma("sp", caus_add[:], caus_add_d, (), ["caus_add"])
            dma("sp", causT[:], causT_d, (), ["causT"])
            dma("sp", invc[:], invc_d, (), ["invc"])
            wload(widx_w[:], w_widx_d, "widx_w", view(72 * 1024, F32, [8, 8]), ["stg_widx"])
            wload(wuk[:], w_uk_d, "wuk", view(80 * 1024, F32, [2, 1024]), ["stg_wuk"])
            wload(wuv[:], w_uv_d, "wuv", view(88 * 1024, F32, [2, 1024]), ["stg_wuv"])
            memset("pool", onesb[:], 1.0, ["onesb"])
            memset("pool", cst[:, 0:1], 1e-6, ["cst"])
            memset("pool", cst[:, 1:2], 1e-5, ["cst"])
            memset("pool", cst[:, 2:3], -1e29, ["cst"])

            hTb_old = view(0, BF16, [8, L])
            Qpair = view(32 * K, BF16, [8, L])
            oT = view(64 * K, BF16, [8, L])
            MOFF = [0, 4, 12, 24]
            maskT = view(96 * K, BF16, [40, 512])
            c_kvT = view(136 * K, BF16, [2, L])
            c_raw = view(144 * K, F32, [2, L])
            csq = view(160 * K, BF16, [2, L])
            k_idxT = view(168 * K, BF16, [L])
            q_idxT = view(172 * K, BF16, [4, L])
            wst = [view(188 * K + i * 2 * K, BF16, [8, 128]) for i in range(2)]
            xs = [view(64 * K + i * 4 * K, F32, [D]) for i in range(2)]
            stgA = [view(96 * K + i * 4 * K, F32, [8, 128]) for i in range(2)]

            for tti in range(NT):
                b = tti % 2
                dma("sp", xs[b], x_d[tti * 128:(tti + 1) * 128, :], (), [("xs", b)])
                for c in range(8):
                    tr(ps[:, 2 * b + c // 4, (c % 4) * 128:(c % 4 + 1) * 128], xs[b][:, c * 128:(c + 1) * 128], ident[:],
                       [("xs", b), "ident"], [("ps", 2 * b + c // 4)])
                act(hTb_old[:, :, tti * 128:(tti + 1) * 128],
                    ps[:, 2 * b:2 * b + 2, :].rearrange("p a (c t) -> p (a c) t", t=128), AF.Copy,
                    [("ps", 2 * b), ("ps", 2 * b + 1)], [("hTbo", tti // 4)])

            bank_rr = [0]

            def nbank(lo=0, n=4):
                b = lo + bank_rr[0] % n
                bank_rr[0] += 1
                return b
            for tix in range(15):
                wb = tix % 2
                wload(wst[wb], w_in_d[tix], ("wst", wb), stgA[wb], [("stgA", wb)])
                for tb in range(NB):
                    bk = nbank()
                    for c in range(8):
                        mm(ps[:, bk, :], wst[wb][:, c, :], hTb_old[:, c, cols(tb)], c == 0, c == 7,
                           [("wst", wb), ("hTbo", tb)], [("ps", bk)])
                    if tix < 8:
                        act(Qpair[:, tix, cols(tb)], ps[:, bk, :], AF.Copy, [("ps", bk)], [("Qpair", tix, tb)], scale=0.125)
                    elif tix < 10:
                        cp("dve", c_raw[:, tix - 8, cols(tb)], ps[:, bk, :], [("ps", bk)], [("c_raw", tb)])
                        act(csq[:, tix - 8, cols(tb)], ps[:, bk, :], AF.Square, [("ps", bk)], [("csq", tb)])
                    elif tix < 14:
                        act(q_idxT[:, tix - 10, cols(tb)], ps[:, bk, :], AF.Copy, [("ps", bk)], [("q_idxT",)])
                    else:
                        cp("dve", k_idxT[:, cols(tb)], ps[:, bk, :], [("ps", bk)], [("k_idxT",)])
            for tti in range(NT):
                for c in range(8):
                    mm(ps[:, 4, tti * 8:(tti + 1) * 8], hTb_old[:, c, tti * 128:(tti + 1) * 128], widx_w[:, c, :],
                       c == 0, c == 7, [("hTbo", tti // 4), "widx_w"], [("ps", 4)], skip=True)
            cp("dve", widx_sb[:], ps[:, 4, 0:128].rearrange("p (a b) -> p a b", a=16), [("ps", 4)], ["widx_sb"])

            sd_a = xs[0][:, 0:512]
            rstd_a = xs[1][:, 0:512]
            for tb in range(NB):
                bk = 5
                for cc in range(2):
                    mm(ps[:, bk, :], onesb[:], csq[:, cc, cols(tb)], cc == 0, cc == 1, ["onesb", ("csq", tb)], [("ps", bk)])
                act(sd_a, ps[:, bk, :], AF.Sqrt, [("ps", bk), "cst"], [("xs", 0)],
                    bias=cst[:, 0:1], scale=1.0 / 256)
                P.add("dve", lambda h: h.reciprocal(out=rstd_a, in_=sd_a), [("xs", 0)], [("xs", 1)])
                for cc in range(2):
                    stt("dve", c_kvT[:, cc, cols(tb)], c_raw[:, cc, cols(tb)], kvg[:, cc:cc + 1], rstd_a, ALU.mult, ALU.mult,
                        [("c_raw", tb), "kvg", ("xs", 1)], [("c_kvT", tb)])
            emit_phase()

            acc = [view(i * 8 * K, F32, [L]) for i in range(4)]
            A3O = 144 * K
            rbuf = [view(A3O + i * 2 * K, F32, [512]) for i in range(4)]
            junk = view(A3O + 8 * K, BF16, [L])
            mask_qs = [view(A3O + 12 * K + i * 4 * K, BF16, [L]) for i in range(2)]
            tbank = [0]
            F_LO, F_MX, F_W, F_HW, F_MID, F_CNT, F_STEP, F_NMID, F_S = range(9)
            junk2 = view(A3O + 20 * K, BF16, [L])

            def scf(f, c0=0, c1=4):
                return sc[:, f * 4 + c0:f * 4 + c1]
            rcount = 0
            for QB in range(NB):
                for jq in range(4):
                    qt = 4 * QB + jq
                    n = (qt + 1) * 128
                    a = acc[jq]
                    atok = ("acc", jq)
                    nsb = (n + 511) // 512
                    for sbk in range(nsb):
                        w = min(512, n - sbk * 512)
                        for hi in range(8):
                            r0 = (hi % 2) * 64
                            bk = nbank()
                            mm(ps[:, bk, 0:w], q_idxT[r0:r0 + 64, hi // 2, qt * 128:(qt + 1) * 128],
                               k_idxT[r0:r0 + 64, sbk * 512:sbk * 512 + w], True, True,
                               [("q_idxT",), ("k_idxT",)], [("ps", bk)])
                            rb = rcount % 4
                            rcount += 1
                            act(rbuf[rb][:, 0:w], ps[:, bk, 0:w], AF.Relu, [("ps", bk)], [("rbuf", rb)])
                            if hi == 0:
                                ts("dve", a[:, sbk * 512:sbk * 512 + w], rbuf[rb][:, 0:w], widx_sb[:, qt, 0:1], None, ALU.mult, None,
                                   [("rbuf", rb), "widx_sb"], [atok])
                            else:
                                stt("dve", a[:, sbk * 512:sbk * 512 + w], rbuf[rb][:, 0:w], widx_sb[:, qt, hi:hi + 1],
                                    a[:, sbk * 512:sbk * 512 + w], ALU.mult, ALU.add,
                                    [("rbuf", rb), "widx_sb", atok], [atok])
                    if qt >= 2:
                        P.add("dve", lambda h, a=a, qt=qt, jq=jq: h.tensor_reduce(out=scf(F_LO, jq, jq + 1), in_=a[:, 0:qt * 128],
                                                                                  axis=AX.X, op=ALU.min), [atok], ["sc_lo"])
                        P.add("dve", lambda h, a=a, n=n, jq=jq: h.tensor_reduce(out=scf(F_MX, jq, jq + 1), in_=a[:, 0:n],
                                                                                axis=AX.X, op=ALU.max), [atok], ["sc_mx"])
                    tt("dve", a[:, qt * 128:(qt + 1) * 128], a[:, qt * 128:(qt + 1) * 128], caus_add[:], ALU.add,
                       [atok, "caus_add", "sc_mx"], [atok])
                c0 = 2 if QB == 0 else 0
                tt("dve", scf(F_W, c0), scf(F_MX, c0), scf(F_LO, c0), ALU.subtract, ["sc_lo", "sc_mx"], ["sc_w"])
                act_cols = (1, 3) if c0 == 0 else ()
                for it in range(NIT):
                    f = 2.0 ** -(it + 1)
                    ts("dve", scf(F_HW, c0), scf(F_W, c0), f, None, ALU.mult, None, ["sc_w"], ["sc_hw"])
                    tt("dve", scf(F_MID, c0), scf(F_LO, c0), scf(F_HW, c0), ALU.add, ["sc_lo", "sc_hw"], ["sc_mid"])
                    if act_cols:
                        ts("dve", scf(F_NMID, c0), scf(F_MID, c0), -1.0, None, ALU.mult, None, ["sc_mid"], ["sc_nmid"])
                    for jq in act_cols:
                        n = (4 * QB + jq + 1) * 128
                        P.add("act", lambda h, jq=jq, n=n: h.activation(
                            out=junk2[:, 0:n], in_=acc[jq][:, 0:n], func=AF.Sign, bias=scf(F_NMID, jq, jq + 1), scale=1.0,
                            accum_out=scf(F_S, jq, jq + 1)), [("acc", jq), "sc_nmid"], ["junk2", ("sc_S", jq)])
                    for jq in range(c0, 4):
                        if jq in act_cols:
                            continue
                        n = (4 * QB + jq + 1) * 128
                        ts("dve", junk[:, 0:n], acc[jq][:, 0:n], scf(F_MID, jq, jq + 1), None, ALU.is_ge, ALU.add,
                           [("acc", jq), "sc_mid"], ["junk", "sc_cnt"], accum=scf(F_CNT, jq, jq + 1))
                    for jq in act_cols:
                        n = (4 * QB + jq + 1) * 128
                        ts("dve", scf(F_CNT, jq, jq + 1), scf(F_S, jq, jq + 1), 0.5, 0.5 * n, ALU.mult, ALU.add,
                           [("sc_S", jq)], ["sc_cnt"])
                    stt("dve", scf(F_STEP, c0), scf(F_CNT, c0), 255.5, scf(F_HW, c0), ALU.is_gt, ALU.mult,
                        ["sc_cnt", "sc_hw"], ["sc_step"])
                    tt("dve", scf(F_LO, c0), scf(F_LO, c0), scf(F_STEP, c0), ALU.add, ["sc_lo", "sc_step"], ["sc_lo"])
                for jq in range(4):
                    qt = 4 * QB + jq
                    n = (qt + 1) * 128
                    if qt < 2:
                        lo_ap, lo_tok = cst[:, 2:3], "cst"
                    else:
                        lo_ap, lo_tok = scf(F_LO, jq, jq + 1), "sc_lo"
                    mq = mask_qs[jq % 2]
                    mtok = ("mask_q", jq % 2)
                    ts("dve", mq[:, 0:n], acc[jq][:, 0:n], lo_ap, None, ALU.is_ge, None, [("acc", jq), lo_tok], [mtok])
                    for k0 in range(0, qt + 1, 4):
                        nk = min(4, qt + 1 - k0)
                        bk = 4 + tbank[0] % 2
                        tbank[0] += 1
                        for i in range(nk):
                            tr(psb(bk)[:, i * 128:(i + 1) * 128], mq[:, (k0 + i) * 128:(k0 + i + 1) * 128], identb[:],
                               [mtok, "identb"], [("ps", bk)])
                        act(maskT[:, MOFF[QB] + k0:MOFF[QB] + k0 + nk, jq * 128:(jq + 1) * 128],
                            psb(bk)[:, 0:nk * 128].rearrange("p (a b) -> p a b", a=nk), AF.Copy,
                            [("ps", bk)], [("maskT", QB)])
            emit_phase()

            BO = 144 * K
            KA = [view(BO + i * 4 * K, BF16, [L]) for i in range(4)]
            Vg = view(BO + 16 * K, BF16, [16, 4, 65])
            QA = [[view(BO + 26 * K + (b * 4 + i) * K, BF16, [512]) for i in range(4)] for b in range(2)]
            ptb = [view(BO + 34 * K + i * K, BF16, [512]) for i in range(8)]
            otok = [view(BO + 42 * K + i * 2 * K, BF16, [4, 256]) for i in range(2)]
            for i in range(4):
                if i % 2 == 1:
                    memset("pool", KA[i][0:64, :], 0.0, [("KA", i)])
                    dma("sp", KA[i][32:40, :], posrows_d, (), [("KA", i)])
                    for b in range(2):
                        memset("pool", QA[b][i][0:64, :], 0.0, [("QA", b, i)])
                else:
                    dma("sp", KA[i][64:72, :], posrows_d, (), [("KA", i)])
            memset("pool", Vg[:, :, :, 64:65], 1.0, ["Vg"])
            hcount = 0
            for g in range(4):
                for pr in range(2):
                    ptile = 2 * g + pr
                    for tb in range(NB):
                        bk = 4 + (pr * 4 + tb) % 2
                        for cc in range(2):
                            mm(ps[:, bk, :], wuk[:, cc, ptile * 128:(ptile + 1) * 128], c_kvT[:, cc, cols(tb)], cc == 0, cc == 1,
                               ["wuk", ("c_kvT", tb)], [("ps", bk)])
                        act(KA[2 * pr][0:64, cols(tb)], ps[0:64, bk, :], AF.Copy, [("ps", bk)], [("KA", 2 * pr)])
                        cp("dve", KA[2 * pr + 1][64:128, cols(tb)], ps[64:128, bk, :], [("ps", bk)], [("KA", 2 * pr + 1)])
                for s_t in range(NT):
                    bk = 4 + s_t % 2
                    for cc in range(2):
                        mm(ps[:, bk, 0:256], c_kvT[:, cc, s_t * 128:(s_t + 1) * 128], wuv[:, cc, g * 256:(g + 1) * 256],
                           cc == 0, cc == 1, ["wuv", ("c_kvT", s_t // 4)], [("ps", bk)])
                    src = ps[:, bk, 0:256].rearrange("p (a b) -> p a b", a=4)
                    if s_t % 2 == 0:
                        act(Vg[:, s_t, :, 0:64], src, AF.Copy, [("ps", bk)], ["Vg"])
                    else:
                        cp("dve", Vg[:, s_t, :, 0:64], src, [("ps", bk)], ["Vg"])
                items = [(QB, hl, kt) for QB in range(NB) for hl in range(4) for kt in range(4 * QB + 4)]
                LA = 5
                obank_of = {}
                pb_of = {}

                def front(i):
                    QB, hl, kt = items[i]
                    qb = (g * 4 + QB) % 2
                    if hl == 0 and kt == 0:
                        for h2 in range(4):
                            h_abs = 4 * g + h2
                            ptile = 2 * g + h2 // 2
                            if h2 % 2 == 0:
                                cp("pool", QA[qb][h2][0:64, :], Qpair[0:64, ptile, cols(QB)],
                                   [("Qpair", ptile, QB)], [("QA", qb, h2)])
                                dma("sp", QA[qb][h2][64:72, :], qcoef_d[h_abs, :, cols(QB)], (), [("QA", qb, h2)])
                            else:
                                cp("pool", QA[qb][h2][64:128, :], Qpair[64:128, ptile, cols(QB)],
                                   [("Qpair", ptile, QB)], [("QA", qb, h2)])
                                dma("sp", QA[qb][h2][32:40, :], qcoef_d[h_abs, :, cols(QB)], (), [("QA", qb, h2)])
                    kr = 72 if hl % 2 == 0 else 128
                    j0 = max(0, kt - 4 * QB)
                    sbank = nbank(0, 6)
                    mm(ps[:, sbank, j0 * 128:512], KA[hl][0:kr, kt * 128:(kt + 1) * 128], QA[qb][hl][0:kr, j0 * 128:512],
                       True, kt < 4 * QB, [("KA", hl), ("QA", qb, hl)], [("ps", sbank)])
                    if kt >= 4 * QB:
                        mm(ps[:, sbank, j0 * 128:(j0 + 1) * 128], identb[:], causT[:], False, True,
                           ["identb", "causT"], [("ps", sbank)])
                    pb = i % 8
                    pb_of[i] = pb
                    act(ptb[pb][:, j0 * 128:512], ps[:, sbank, j0 * 128:512], AF.Exp, [("ps", sbank)], [("ptb", pb)])
                    tt("dve", ptb[pb][:, j0 * 128:512], ptb[pb][:, j0 * 128:512],
                       maskT[:, MOFF[QB] + kt, j0 * 128:512], ALU.mult, [("ptb", pb), ("maskT", QB)], [("ptb", pb)])

                def back(i):
                    nonlocal hcount
                    QB, hl, kt = items[i]
                    ob = (g * 4 + QB) % 2
                    if kt == 0:
                        obank_of[(QB, hl)] = 6 + hcount % 2
                        hcount += 1
                    obank = obank_of[(QB, hl)]
                    psO = ps[:, obank, :].rearrange("p (j f) -> p j f", j=4)
                    j0 = max(0, kt - 4 * QB)
                    pb = pb_of[i]
                    for j in range(j0, 4):
                        mm(psO[:, j, 0:65], ptb[pb][:, j * 128:(j + 1) * 128], Vg[:, kt, hl, :],
                           kt == 0 and j == 0, kt == 4 * QB + j, [("ptb", pb), "Vg"], [("ps", obank)], skip=True)
                    if kt == 4 * QB + 3:
                        ri = obank - 6
                        rd = rden[:, ri, :]
                        ts("dve", rd, psO[:, :, 64], 1e-30, None, ALU.add, None, [("ps", obank)], [("rden", ri)])
                        P.add("dve", lambda h, rd=rd: h.reciprocal(out=rd, in_=rd), [("rden", ri)], [("rden", ri)])
                        for j in range(4):
                            ts("dve", otok[ob][:, j, hl * 64:(hl + 1) * 64], psO[:, j, 0:64], rden[:, ri, j:j + 1], None,
                               ALU.mult, None, [("ps", obank), ("rden", ri)], [("otok", ob)])
                        if hl == 3:
                            for cc in range(2):
                                bk = 4 + cc
                                for j in range(4):
                                    tr(psb(bk)[:, j * 128:(j + 1) * 128], otok[ob][:, j, cc * 128:(cc + 1) * 128], identb[:],
                                       [("otok", ob), "identb"], [("ps", bk)])
                                act(oT[:, 2 * g + cc, cols(QB)], psb(bk)[:, 0:512], AF.Copy, [("ps", bk)], [("oT", QB)])

                for step in range(len(items) + LA):
                    if step < len(items):
                        front(step)
                    if step - LA >= 0:
                        back(step - LA)
            emit_phase()

            hT = view(96 * K, F32, [8, L])
            hTb = view(0, BF16, [8, L])
            SO = 160 * K
            wst2 = [view(SO + i * 2 * K, BF16, [8, 128]) for i in range(2)]
            xs2 = [view(SO + 4 * K + i * 4 * K, F32, [D]) for i in range(2)]
            zb = [view(SO + 12 * K + i * K, BF16, [512]) for i in range(2)]
            zsq = [view(SO + 14 * K + i * K, BF16, [512]) for i in range(2)]
            mean_t = view(SO + 16 * K, F32, [512])
            msq_t = view(SO + 18 * K, F32, [512])
            rstd_t = view(SO + 20 * K, F32, [512])
            tbuf = [view(SO + 22 * K + i * 2 * K, F32, [512]) for i in range(2)]
            sgb = [view(SO + 26 * K + i * 2 * K, F32, [512]) for i in range(2)]
            actT = view(32 * K, BF16, [NJ, 1024])
            wdn = [view(76 * K + i * 6 * K, BF16, [NJ, 128]) for i in range(2)]
            wgu = [view(88 * K + i * 2 * K, BF16, [8, 128]) for i in range(4)]
            gst = [view(SO + i * 4 * K, F32, [8, 128]) for i in range(3)]
            dst_ = view(SO, F32, [NJ, 128])
            gcount = [0]
            lcount = [0]

            def layer_norm(tb, lyr, which, final_bf16=True):
                gi, bi = (0, 1) if which == 0 else (2, 3)
                for c in range(8):
                    k = lcount[0] % 2
                    lcount[0] += 1
                    cp("dve", zb[k], hT[:, c, cols(tb)], [("hT", c, tb)], [("zb", k)])
                    act(zsq[k], hT[:, c, cols(tb)], AF.Square, [("hT", c, tb)], [("zsq", k)])
                    mm(ps[:, 6, :], onesb[:], zb[k], c == 0, c == 7, ["onesb", ("zb", k)], [("ps", 6)])
                    mm(ps[:, 7, :], onesb[:], zsq[k], c == 0, c == 7, ["onesb", ("zsq", k)], [("ps", 7)])
                ts("dve", mean_t, ps[:, 6, :], 1.0 / D, None, ALU.mult, None, [("ps", 6)], ["mean_t"])
                tt("dve", msq_t, mean_t, mean_t, ALU.mult, ["mean_t"], ["msq_t"])
                stt("dve", msq_t, ps[:, 7, :], 1.0 / D, msq_t, ALU.mult, ALU.subtract, [("ps", 7), "msq_t"], ["msq_t"])
                act(rstd_t, msq_t, AF.Sqrt, ["msq_t", "cst"], ["rstd_t"], bias=cst[:, 1:2], scale=1.0)
                P.add("dve", lambda h: h.reciprocal(out=rstd_t, in_=rstd_t), ["rstd_t"], ["rstd_t"])
                for c in range(8):
                    k = lcount[0] % 2
                    lcount[0] += 1
                    tt("dve", tbuf[k], hT[:, c, cols(tb)], mean_t, ALU.subtract, [("hT", c, tb), "mean_t"], [("tbuf", k)])
                    tt("dve", tbuf[k], tbuf[k], rstd_t, ALU.mult, [("tbuf", k), "rstd_t"], [("tbuf", k)])
                    act(hT[:, c, cols(tb)], tbuf[k], AF.Identity, [("tbuf", k), "lnp"], [("hT", c, tb)],
                        bias=lnp[:, lyr, bi, c:c + 1], scale=lnp[:, lyr, gi, c:c + 1])
                    if final_bf16:
                        ts("pool", hTb[:, c, cols(tb)], tbuf[k], lnp[:, lyr, gi, c:c + 1], lnp[:, lyr, bi, c:c + 1],
                           ALU.mult, ALU.add, [("tbuf", k), "lnp"], [("hTb", c, tb)])

            for tti in range(NT):
                b = tti % 2
                dma("sp", xs2[b], x_d[tti * 128:(tti + 1) * 128, :], (), [("xs2", b)])
                for c in range(8):
                    tr(ps[:, 2 * b + c // 4, (c % 4) * 128:(c % 4 + 1) * 128], xs2[b][:, c * 128:(c + 1) * 128], ident[:],
                       [("xs2", b), "ident"], [("ps", 2 * b + c // 4)])
                act(hT[:, :, tti * 128:(tti + 1) * 128],
                    ps[:, 2 * b:2 * b + 2, :].rearrange("p a (c t) -> p (a c) t", t=128), AF.Copy,
                    [("ps", 2 * b), ("ps", 2 * b + 1)], [("hT", c, tti // 4) for c in range(8)], scale=ALPHA)

            def out_proj(w_d, src, srctok, wst2=wst2, stg=None, alpha=None):
                for it in range(8):
                    wb = it % 2
                    if stg is None:
                        wload(wst2[wb], w_d[it], ("wst3", wb), xs2[wb].rearrange("p (a b) -> p a b", a=8), [("xs2", wb)])
                    else:
                        wload(wst2[wb], w_d[it], ("wst3", wb), stg, ["stgE"])
                    for tb in range(NB):
                        bk = nbank(0, 4)
                        for c in range(8):
                            mm(ps[:, bk, :], wst2[wb][:, c, :], src[:, c, cols(tb)], c == 0, c == 7,
                               [("wst3", wb), (srctok, tb)], [("ps", bk)])
                        if alpha is None:
                            tt("dve", hT[:, it, cols(tb)], hT[:, it, cols(tb)], ps[:, bk, :], ALU.add,
                               [("ps", bk), ("hT", it, tb)], [("hT", it, tb)])
                        else:
                            stt("dve", hT[:, it, cols(tb)], hT[:, it, cols(tb)], alpha, ps[:, bk, :], ALU.mult, ALU.add,
                                [("ps", bk), ("hT", it, tb)], [("hT", it, tb)])
            out_proj(a_wo_d, oT, "oT")
            for tb in range(NB):
                layer_norm(tb, 0, 0)
            snapshot(hT)
            emit_phase()

            def ffn(lyr, last):
                def load_gu(jt):
                    wb = jt % 2
                    k0 = gcount[0] % 3
                    k1 = (gcount[0] + 1) % 3
                    gcount[0] += 2
                    wload(wgu[wb], w_gu_d[lyr, jt], ("wgu", wb), gst[k0], [("gst", k0)], ceng="act")
                    wload(wgu[2 + wb], w_gu_d[lyr, NJ + jt], ("wgu", 2 + wb), gst[k1], [("gst", k1)], ceng="pool")

                def load_dn(it):
                    wb = it % 2
                    toks = [("gst", 0), ("gst", 1), ("gst", 2)]
                    dma("sp", dst_, w_dn_d[lyr, it], (), toks)
                    act(wdn[wb][:, 0:11, :], dst_[:, 0:11, :], AF.Copy, toks, [("wdn", wb, 0)])
                    cp("pool", wdn[wb][:, 11:NJ, :], dst_[:, 11:NJ, :], toks, [("wdn", wb, 1)])

                for half in range(2):
                    load_gu(0)
                    for jt in range(NJ):
                        wb = jt % 2
                        if jt + 1 < NJ:
                            load_gu(jt + 1)
                        for t2 in range(2):
                            tb = 2 * half + t2
                            bg = nbank(0, 4)
                            bu = nbank(0, 4)
                            for c in range(8):
                                mm(ps[:, bg, :], wgu[wb][:, c, :], hTb[:, c, cols(tb)], c == 0, c == 7,
                                   [("wgu", wb), ("hTb", c, tb)], [("ps", bg)])
                            for c in range(8):
                                mm(ps[:, bu, :], wgu[2 + wb][:, c, :], hTb[:, c, cols(tb)], c == 0, c == 7,
                                   [("wgu", 2 + wb), ("hTb", c, tb)], [("ps", bu)])
                            k = (jt * 2 + t2) % 2
                            act(sgb[k], ps[:, bg, :], AF.Silu, [("ps", bg)], [("sgb", k)])
                            tt("dve", actT[:, jt, t2 * 512:(t2 + 1) * 512], sgb[k], ps[:, bu, :], ALU.mult,
                               [("sgb", k), ("ps", bu)], [("actT", t2)])
                    load_dn(0)
                    for it in range(8):
                        wb = it % 2
                        if it + 1 < 8:
                            load_dn(it + 1)
                        for t2 in range(2):
                            tb = 2 * half + t2
                            bk = nbank(0, 4)
                            for jt in range(NJ):
                                mm(ps[:, bk, :], wdn[wb][:, jt, :], actT[:, jt, t2 * 512:(t2 + 1) * 512], jt == 0, jt == NJ - 1,
                                   [("wdn", wb, 0), ("wdn", wb, 1), ("actT", t2)], [("ps", bk)])
                            stt("dve", hT[:, it, cols(tb)], hT[:, it, cols(tb)], ALPHA, ps[:, bk, :], ALU.mult, ALU.add,
                                [("ps", bk), ("hT", it, tb)], [("hT", it, tb)])
                    for t2 in range(2):
                        layer_norm(2 * half + t2, lyr, 1, final_bf16=not last)
                snapshot(hT)
                emit_phase()
            ffn(0, False)

            pooledT = view(32 * K, BF16, [8, L])
            ysT = view(64 * K, BF16, [8, L])
            UP = (16 + L) * 4
            upad = [view(64 * K + i * UP, F32, [16 + L]) for i in range(2)]
            sA = view(64 * K + 2 * UP, F32, [16 + L])
            sB = view(SO + 2 * UP, F32, [16 + L])
            wst3 = [view(SO + 3 * UP + i * 2 * K, BF16, [8, 128]) for i in range(2)]
            wgr = view(SO + 3 * UP + 4 * K, BF16, [2, 256])
            fix = view(SO + 3 * UP + 5 * K, F32, [16])
            stgE = view(SO + 3 * UP + 6 * K, F32, [8, 128])
            stgE2 = view(SO + 3 * UP + 6 * K, F32, [2, 256])
            for t_ in (upad[0], upad[1], sA, sB):
                memset("pool", t_[:, 0:16], 0.0, ["pads"])
            for mt in range(8):
                gi = mt // 2
                w = WINS[gi]
                wb = mt % 2
                u = upad[wb]
                utok = ("upad", wb)
                wload(wst3[wb], b_win_d[mt], ("wst3", wb), stgE, ["stgE"])
                for tb in range(NB):
                    bk = nbank(0, 4)
                    for c in range(8):
                        mm(ps[:, bk, :], wst3[wb][:, c, :], hTb[:, c, cols(tb)], c == 0, c == 7,
                           [("wst3", wb), ("hTb", c, tb)], [("ps", bk)])
                    act(u[:, 16 + tb * 512:16 + (tb + 1) * 512], ps[:, bk, :], AF.Copy, [("ps", bk), "pads"], [utok])
                tt("dve", sA[:, 16:], u[:, 16:], u[:, 15:15 + L], ALU.add, [utok, "pads"], ["sA"])
                cur, curtok = sA, "sA"
                if w >= 4:
                    tt("dve", sB[:, 16:], sA[:, 16:], sA[:, 14:14 + L], ALU.add, ["sA", "pads"], ["sB"])
                    cur, curtok = sB, "sB"
                if w >= 8:
                    tt("dve", sA[:, 16:], sB[:, 16:], sB[:, 12:12 + L], ALU.add, ["sB", "pads"], ["sA"])
                    cur, curtok = sA, "sA"
                if w >= 16:
                    tt("dve", sB[:, 16:], sA[:, 16:], sA[:, 8:8 + L], ALU.add, ["sA", "pads"], ["sB"])
                    cur, curtok = sB, "sB"
                stt("dve", pooledT[:, mt, :], cur[:, 16:], 1.0 / w, u[:, 16:], ALU.mult, ALU.subtract,
                    [curtok, utok], [("pooledT", mt)])
                tt("dve", fix[:, 0:16], cur[:, 16:32], invc[:, gi, :], ALU.mult, [curtok, "invc"], ["fix"])
                tt("dve", pooledT[:, mt, 0:16], fix[:, 0:16], u[:, 16:32], ALU.subtract, ["fix", utok], [("pooledT", mt)])
            emit_phase()
            for gi in range(4):
                wload(wgr, b_wgrp_d[gi], "wgr", stgE2, ["stgE"])
                for dt_ in range(2):
                    mo = 2 * gi + dt_
                    for tb in range(NB):
                        bk = nbank(0, 4)
                        for cc in range(2):
                            mm(ps[:, bk, :], wgr[:, cc, dt_ * 128:(dt_ + 1) * 128], pooledT[:, 2 * gi + cc, cols(tb)],
                               cc == 0, cc == 1, ["wgr", ("pooledT", 2 * gi + cc)], [("ps", bk)])
                        act(ysT[:, mo, cols(tb)], ps[:, bk, :], AF.Identity, [("ps", bk), "bscale"], [("ysT", tb)],
                            scale=bscale[:, mo:mo + 1])
            out_proj(b_wo_d, ysT, "ysT", wst2=wst3, stg=stgE, alpha=ALPHA)
            for tb in range(NB):
                layer_norm(tb, 1, 0)
            snapshot(hT)
            emit_phase()

            ffn(1, True)

            for tti in range(NT):
                b = tti % 2
                for c in range(8):
                    tr(ps[:, 2 * b + c // 4, (c % 4) * 128:(c % 4 + 1) * 128], hT[:, c, tti * 128:(tti + 1) * 128], ident[:],
                       [("hT", c, tti // 4), "ident"], [("ps", 2 * b + c // 4)])
                src = ps[:, 2 * b:2 * b + 2, :].rearrange("p a f -> p (a f)")
                if b == 0:
                    cp("dve", xs2[b], src, [("ps", 2 * b), ("ps", 2 * b + 1)], [("xs2", b)])
                else:
                    act(xs2[b], src, AF.Copy, [("ps", 2 * b), ("ps", 2 * b + 1)], [("xs2", b)])
                dma("sp", out_d[tti * 128:(tti + 1) * 128, :], xs2[b], [("xs2", b)], [("out", tti)])
            emit_phase()

        except _Stop:
            pass
    return nc


def _alibi_slopes():
    return np.exp2(-8.0 * np.arange(1, 17, dtype=np.float64) / 16.0)


def _host_consts():
    bf = ml_dtypes.bfloat16
    c = {}
    c["ident"] = np.eye(128, dtype=np.float32)
    c["identb"] = np.eye(128, dtype=np.float32).astype(bf)
    s = np.arange(L)
    pos = np.zeros((8, L), np.float32)
    pos[0] = s // 16
    pos[1] = s // 16
    pos[2] = s % 16
    pos[3] = s % 16
    pos[4] = 1.0
    c["posrows"] = pos.astype(bf)
    sl = _alibi_slopes()
    qc = np.zeros((16, 8, L), np.float32)
    for h in range(16):
        hi = np.float32(sl[h]).astype(bf).astype(np.float32)
        lo = np.float32(sl[h] - float(hi)).astype(bf).astype(np.float32)
        qc[h, 0] = 16.0 * hi
        qc[h, 1] = 16.0 * lo
        qc[h, 2] = hi
        qc[h, 3] = lo
        qc[h, 4] = -(sl[h] * s)
    c["qcoef"] = qc.astype(bf)
    qi = np.arange(128)[:, None]
    si = np.arange(128)[None, :]
    c["caus_add"] = np.where(si <= qi, 0.0, -1e30).astype(np.float32)
    c["causT"] = np.where(qi <= si, 0.0, -30000.0).astype(np.float32).astype(bf)
    invc = np.zeros((128, 4, 16), np.float32)
    for gi, w in enumerate(WINS):
        invc[:, gi, :] = 1.0 / np.minimum(w, np.arange(16) + 1)
    c["invc"] = invc
    return c


def _tiles_kc(w, ncols_tile=128):
    kd, n = w.shape
    return np.ascontiguousarray(w.reshape(kd // 128, 128, n // ncols_tile, ncols_tile).transpose(2, 1, 0, 3))


def _vec_pc(v):
    return np.ascontiguousarray(v.reshape(-1, 128).T)


def _prep_weights(a_w_in, a_w_uk, a_w_uv, a_kv_norm_g, a_w_o, b_w_in, b_w_grp, b_scale, b_w_o,
                  f_w_gu, f_w_down, ln_mix_g, ln_mix_b, ln_ffn_g, ln_ffn_b):
    m = {}
    w_in = a_w_in[0]
    colsel = np.concatenate([np.arange(0, 1792), np.arange(1792, 1856), np.arange(1792, 1856)])
    m["w_in_t"] = _tiles_kc(w_in[:, colsel])
    m["w_widx"] = np.ascontiguousarray(w_in[:, 1856:1864].reshape(8, 128, 8).transpose(1, 0, 2))
    m["w_uk_t"] = np.ascontiguousarray(a_w_uk[0].transpose(1, 0, 2).reshape(2, 128, 1024).transpose(1, 0, 2))
    m["w_uv_t"] = np.ascontiguousarray(a_w_uv[0].transpose(1, 0, 2).reshape(2, 128, 1024).transpose(1, 0, 2))
    m["kvg"] = _vec_pc(a_kv_norm_g[0])
    m["a_wo_t"] = _tiles_kc(a_w_o[0])
    m["b_win_t"] = _tiles_kc(b_w_in[0])
    m["b_wgrp_t"] = np.ascontiguousarray(b_w_grp[0].reshape(4, 2, 128, 256).transpose(0, 2, 1, 3))
    m["b_scale_t"] = _vec_pc(b_scale[0])
    m["b_wo_t"] = _tiles_kc(b_w_o[0])
    m["w_gu_t"] = np.stack([_tiles_kc(f_w_gu[i]) for i in range(2)])
    m["w_dn_t"] = np.stack([_tiles_kc(f_w_down[i]) for i in range(2)])
    lnp = np.zeros((128, 2, 4, 8), np.float32)
    for i in range(2):
        lnp[:, i, 0] = _vec_pc(ln_mix_g[i])
        lnp[:, i, 1] = _vec_pc(ln_mix_b[i])
        lnp[:, i, 2] = _vec_pc(ln_ffn_g[i])
        lnp[:, i, 3] = _vec_pc(ln_ffn_b[i])
    m["lnp"] = lnp
    return {k: np.ascontiguousarray(v, dtype=np.float32) for k, v in m.items()}


_NC_CACHE = {}


def kernel(x, a_w_in, a_w_uk, a_w_uv, a_kv_norm_g, a_w_o, b_w_in, b_w_grp, b_scale, b_w_o,
           f_w_gu, f_w_down, ln_mix_g, ln_mix_b, ln_ffn_g, ln_ffn_b, _debug=False, _stop=99, _ncores=8):
    f = lambda a: np.asarray(a, dtype=np.float32)
    wm = _prep_weights(f(a_w_in), f(a_w_uk), f(a_w_uv), f(a_kv_norm_g), f(a_w_o), f(b_w_in), f(b_w_grp),
                       f(b_scale), f(b_w_o), f(f_w_gu), f(f_w_down), f(ln_mix_g), f(ln_mix_b), f(ln_ffn_g), f(ln_ffn_b))
    wm.update(_host_consts())
    x = f(x)
    ncores = _ncores
    key = (_debug, _stop)
    if key not in _NC_CACHE:
        _NC_CACHE[key] = build_nc(debug=_debug, stop=_stop)
    nc = _NC_CACHE[key]
    in_maps = []
    for i in range(ncores):
        d = dict(wm)
        d["x"] = np.ascontiguousarray(x[i])
        in_maps.append(d)
    res = run_bass_kernel_spmd(nc, in_maps, core_ids=list(range(ncores)))
    out = np.stack([np.asarray(r["out"], dtype=np.float32) for r in res.results], axis=0)
    if _debug:
        return out, [r["dbg"] for r in res.results]
    return out
```

```python
import contextlib
import numpy as np
import ml_dtypes
import concourse.bass as bass
import concourse.mybir as mybir
from concourse.bass_utils import run_bass_kernel_spmd

F32 = mybir.dt.float32
BF16 = mybir.dt.bfloat16
ALU = mybir.AluOpType
AF = mybir.ActivationFunctionType
AX = mybir.AxisListType

L = 2048
D = 1024
NT = 16
NB = 4
NC8 = 8
DFF = 2816
NJ = 22
ALPHA = 4.0 ** 0.25
WINS = (2, 4, 8, 16)
NIT = 12
DEBUG = False
USE_SWDGE = False


class Op:
    __slots__ = ("eng", "fn", "deps", "ddeps", "idx", "dma", "dsem", "dval", "inc", "cnt", "waits", "dwaits")

    def __init__(self, eng, fn, deps, ddeps, idx, dma):
        self.eng, self.fn, self.deps, self.ddeps, self.idx, self.dma = eng, fn, deps, ddeps, idx, dma
        self.dsem = None
        self.dval = 0
        self.inc = False
        self.cnt = 0
        self.waits = []
        self.dwaits = []


class Prog:
    ENGS = ("pe", "act", "dve", "pool", "sp")
    RING = 8

    def __init__(self, nc, stack):
        self.nc = nc
        self.sems = {e: stack.enter_context(nc.semaphore("s_" + e)) for e in self.ENGS}
        self.dsems = {e: [stack.enter_context(nc.semaphore("d_%s%d" % (e, i))) for i in range(self.RING)]
                      for e in ("sp", "pool")}
        self.ndma = {e: 0 for e in ("sp", "pool")}
        self.count = {e: 0 for e in self.ENGS}
        self.reset_phase()

    def reset_phase(self):
        self.ops = {e: [] for e in self.ENGS}
        self.last_w = {}
        self.readers = {}
        self.phase_dma = {}

    def add(self, eng, fn, reads=(), writes=(), dma=False):
        writes = list(writes) + [t for t in reads if isinstance(t, tuple) and t[0] == "ps" and t not in writes]
        deps, ddeps = {}, {}

        def dep(ev):
            if ev[0] == "d":
                k = (ev[1], ev[2])
                if ddeps.get(k, 0) < ev[3]:
                    ddeps[k] = ev[3]
            else:
                if deps.get(ev[0], -1) < ev[1]:
                    deps[ev[0]] = ev[1]
        for t in reads:
            if t in self.last_w:
                dep(self.last_w[t])
        for t in writes:
            if t in self.last_w:
                dep(self.last_w[t])
            for r in self.readers.get(t, ()):
                dep(r)
        idx = len(self.ops[eng])
        op = Op(eng, fn, deps, ddeps, idx, dma)
        if dma:
            n = self.ndma[eng]
            self.ndma[eng] = n + 1
            slot = n % self.RING
            op.dsem = (eng, slot)
            op.dval = 16 * (n // self.RING + 1)
            if n >= self.RING:
                k = (eng, slot)
                if ddeps.get(k, 0) < op.dval - 16:
                    ddeps[k] = op.dval - 16
            ev = ("d", eng, slot, op.dval)
            self.phase_dma[(eng, slot)] = op.dval
        else:
            ev = (eng, idx)
        self.ops[eng].append(op)
        for t in reads:
            self.readers.setdefault(t, []).append(ev)
        for t in writes:
            self.last_w[t] = ev
            self.readers[t] = []
        return op

    def emit(self):
        nc = self.nc
        last = {}
        for e in self.ENGS:
            for op in reversed(self.ops[e]):
                if not op.dma:
                    last[e] = op.idx
                    break
        for e in self.ENGS:
            deps = {e2: i for e2, i in last.items() if not (e2 == e and e in ("pe", "sp"))}
            self.ops[e].append(Op(e, lambda h: h.nop(), deps, dict(self.phase_dma), len(self.ops[e]), False))
        for e in self.ENGS:
            waited, dwaited = {}, {}
            for op in self.ops[e]:
                for se, si in op.deps.items():
                    if se == "pe" and e == "pe":
                        continue
                    if waited.get(se, -1) < si:
                        waited[se] = si
                        op.waits.append((se, si))
                        self.ops[se][si].inc = True
                for k, v in op.ddeps.items():
                    if dwaited.get(k, 0) < v:
                        dwaited[k] = v
                        op.dwaits.append((k, v))
        for e in self.ENGS:
            c = self.count[e]
            for op in self.ops[e]:
                if op.inc:
                    c += 1
                op.cnt = c
            self.count[e] = c
        ops, sems, dsems = self.ops, self.sems, self.dsems

        def run(e, h):
            for op in ops[e]:
                for se, si in op.waits:
                    h.wait_ge(sems[se], ops[se][si].cnt)
                for (q, slot), v in op.dwaits:
                    h.wait_ge(dsems[q][slot], v)
                ins = op.fn(h)
                if op.dma:
                    ins.then_inc(dsems[op.dsem[0]][op.dsem[1]], 16)
                elif op.inc:
                    ins.then_inc(sems[e], 1)
        with nc.Block() as block:
            @block.tensor
            def _(h):
                run("pe", h)

            @block.scalar
            def _(h):
                run("act", h)

            @block.vector
            def _(h):
                run("dve", h)

            @block.gpsimd
            def _(h):
                run("pool", h)

            @block.sync
            def _(h):
                run("sp", h)
        self.reset_phase()


def build_nc(debug=False, stop=99):
    hcount = 0
    nc = bass.Bass("TRN2", target_bir_lowering=False)

    def din(name, shape, dt=F32):
        return nc.dram_tensor(name, list(shape), dt, kind="ExternalInput").ap()
    x_d = din("x", [L, D])
    w_in_d = din("w_in_t", [15, 128, 8, 128])
    w_widx_d = din("w_widx", [128, 8, 8])
    w_uk_d = din("w_uk_t", [128, 2, 1024])
    w_uv_d = din("w_uv_t", [128, 2, 1024])
    kvg_d = din("kvg", [128, 2])
    a_wo_d = din("a_wo_t", [8, 128, 8, 128])
    b_win_d = din("b_win_t", [8, 128, 8, 128])
    b_wgrp_d = din("b_wgrp_t", [4, 128, 2, 256])
    b_scale_d = din("b_scale_t", [128, 8])
    b_wo_d = din("b_wo_t", [8, 128, 8, 128])
    w_gu_d = din("w_gu_t", [2, 44, 128, 8, 128])
    w_dn_d = din("w_dn_t", [2, 8, 128, NJ, 128])
    lnp_d = din("lnp", [128, 2, 4, 8])
    ident_d = din("ident", [128, 128])
    identb_d = din("identb", [128, 128], BF16)
    posrows_d = din("posrows", [8, L], BF16)
    qcoef_d = din("qcoef", [16, 8, L], BF16)
    caus_add_d = din("caus_add", [128, 128])
    causT_d = din("causT", [128, 128], BF16)
    invc_d = din("invc", [128, 4, 16])
    out_d = nc.dram_tensor("out", [L, D], F32, kind="ExternalOutput").ap()
    if debug:
        dbg_d = nc.dram_tensor("dbg", [6, 128, 8, L], F32, kind="ExternalOutput").ap()

    with contextlib.ExitStack() as st:
        P = Prog(nc, st)

        def sb(name, shape, dt):
            return st.enter_context(nc.sbuf_tensor(name, list(shape), dt))
        ident = sb("ident_sb", [128, 128], F32)
        identb = sb("identb_sb", [128, 128], BF16)
        onesb = sb("onesb", [128, 128], BF16)
        lnp = sb("lnp_sb", [128, 2, 4, 8], F32)
        kvg = sb("kvg_sb", [128, 2], F32)
        bscale = sb("bscale_sb", [128, 8], F32)
        caus_add = sb("caus_add_sb", [128, 128], F32)
        causT = sb("causT_sb", [128, 128], BF16)
        invc = sb("invc_sb", [128, 4, 16], F32)
        widx_w = sb("widx_w", [128, 8, 8], BF16)
        wuk = sb("wuk_sb", [128, 2, 1024], BF16)
        wuv = sb("wuv_sb", [128, 2, 1024], BF16)
        cst = sb("cst", [128, 8], F32)
        widx_sb = sb("widx_sb", [128, 16, 8], F32)
        sc = sb("sc", [128, 36], F32)
        rden = sb("rden", [128, 2, 4], F32)
        ARENA_B = 196 * 1024
        arena = sb("arena", [128, ARENA_B // 2], BF16)
        ps = st.enter_context(nc.psum_tensor("ps", [128, 8, 512], F32))

        def view(off, dt, shape):
            n = int(np.prod(shape))
            nbytes = n * (4 if dt == F32 else 2)
            assert off % 4 == 0 and off + nbytes <= ARENA_B, (off, nbytes)
            ap = arena[:, off // 2:(off + nbytes) // 2]
            if dt == F32:
                ap = ap.bitcast(F32)
            if len(shape) == 2:
                ap = ap.rearrange("p (a b) -> p a b", a=shape[0])
            elif len(shape) == 3:
                ap = ap.rearrange("p (a b c) -> p a b c", a=shape[0], b=shape[1])
            return ap
        K = 1024

        def psb(bank):
            return ps[:, bank, :].bitcast(BF16)

        def mm(out, lhsT, rhs, start, stop, reads, writes, skip=False):
            P.add("pe", lambda h: h.matmul(out, lhsT, rhs, start=start, stop=stop, skip_group_check=skip),
                  reads, writes)

        def tr(out, in_, idn, reads, writes):
            P.add("pe", lambda h: h.transpose(out, in_, idn), reads, writes)

        def act(out, in_, func, reads, writes, bias=None, scale=None):
            kw = {}
            if bias is not None:
                kw["bias"] = bias
            if scale is not None:
                kw["scale"] = scale
            P.add("act", lambda h: h.activation(out=out, in_=in_, func=func, **kw), reads, writes)

        def tt(eng, out, in0, in1, op, reads, writes):
            P.add(eng, lambda h: h.tensor_tensor(out=out, in0=in0, in1=in1, op=op), reads, writes)

        def ts(eng, out, in0, s1, s2, op0, op1, reads, writes, accum=None):
            if op1 is None:
                P.add(eng, lambda h: h.tensor_scalar(out=out, in0=in0, scalar1=s1, scalar2=None, op0=op0),
                      reads, writes)
            elif accum is None:
                P.add(eng, lambda h: h.tensor_scalar(out=out, in0=in0, scalar1=s1, scalar2=s2, op0=op0, op1=op1),
                      reads, writes)
            else:
                P.add(eng, lambda h: h.tensor_scalar(out=out, in0=in0, scalar1=s1, scalar2=s2, op0=op0, op1=op1,
                                                     accum_out=accum), reads, writes)

        def stt(eng, out, in0, scalar, in1, op0, op1, reads, writes):
            P.add(eng, lambda h: h.scalar_tensor_tensor(out=out, in0=in0, scalar=scalar, in1=in1, op0=op0, op1=op1),
                  reads, writes)

        def cp(eng, out, in_, reads, writes):
            P.add(eng, lambda h: h.tensor_copy(out=out, in_=in_), reads, writes)

        def dma(q, out, in_, reads, writes):
            if q == "pool":
                P.add(q, lambda h: h.dma_start(out=out, in_=in_, max_dma_last_dim=4096), reads, writes, dma=True)
            else:
                P.add(q, lambda h: h.dma_start(out=out, in_=in_), reads, writes, dma=True)

        def wload(dst, src, dst_tok, stage, stage_toks, ceng="pool"):
            if USE_SWDGE:
                dma("pool", dst, src, (), [dst_tok])
            else:
                dma("sp", stage, src, (), list(stage_toks))
                if ceng == "act":
                    act(dst, stage, AF.Copy, list(stage_toks), [dst_tok])
                else:
                    cp("pool", dst, stage, list(stage_toks), [dst_tok])

        def memset(eng, ap, val, writes):
            P.add(eng, lambda h: h.memset(ap, val), (), writes)

        def cols(tb):
            return slice(tb * 512, (tb + 1) * 512)

        dbg_n = [0]
        phase_n = [0]

        class _Stop(Exception):
            pass

        def emit_phase():
            P.emit()
            phase_n[0] += 1
            if phase_n[0] >= stop:
                raise _Stop()

        def snapshot(hT):
            if debug:
                k = dbg_n[0]
                dbg_n[0] += 1
                dma("sp", dbg_d[k], hT, [("hT", c, tb) for c in range(8) for tb in range(4)], [("dbg", k)])

        try:
            dma("sp", ident[:], ident_d, (), ["ident"])
            dma("sp", identb[:], identb_d, (), ["identb"])
            dma("sp", lnp[:], lnp_d, (), ["lnp"])
            dma("sp", kvg[:], kvg_d, (), ["kvg"])
            dma("sp", bscale[:], b_scale_d, (), ["bscale"])
            dma("sp", caus_add[:], caus_add_d, (), ["caus_add"])
            dma("sp", causT[:], causT_d, (), ["causT"])
            dma("sp", invc[:], invc_d, (), ["invc"])
            wload(widx_w[:], w_widx_d, "widx_w", view(72 * 1024, F32, [8, 8]), ["stg_widx"])
            wload(wuk[:], w_uk_d, "wuk", view(80 * 1024, F32, [2, 1024]), ["stg_wuk"])
            wload(wuv[:], w_uv_d, "wuv", view(88 * 1024, F32, [2, 1024]), ["stg_wuv"])
            memset("pool", onesb[:], 1.0, ["onesb"])
            memset("pool", cst[:, 0:1], 1e-6, ["cst"])
            memset("pool", cst[:, 1:2], 1e-5, ["cst"])
            memset("pool", cst[:, 2:3], -1e29, ["cst"])

            hTb_old = view(0, BF16, [8, L])
            Qpair = view(32 * K, BF16, [8, L])
            oT = view(64 * K, BF16, [8, L])
            MOFF = [0, 4, 12, 24]
            maskT = view(96 * K, BF16, [40, 512])
            c_kvT = view(136 * K, BF16, [2, L])
            c_raw = view(144 * K, F32, [2, L])
            csq = view(160 * K, BF16, [2, L])
            k_idxT = view(168 * K, BF16, [L])
            q_idxT = view(172 * K, BF16, [4, L])
            wst = [view(188 * K + i * 2 * K, BF16, [8, 128]) for i in range(2)]
            xs = [view(64 * K + i * 4 * K, F32, [D]) for i in range(2)]
            stgA = [view(96 * K + i * 4 * K, F32, [8, 128]) for i in range(2)]

            for tti in range(NT):
                b = tti % 2
                dma("sp", xs[b], x_d[tti * 128:(tti + 1) * 128, :], (), [("xs", b)])
                for c in range(8):
                    tr(ps[:, 2 * b + c // 4, (c % 4) * 128:(c % 4 + 1) * 128], xs[b][:, c * 128:(c + 1) * 128], ident[:],
                       [("xs", b), "ident"], [("ps", 2 * b + c // 4)])
                act(hTb_old[:, :, tti * 128:(tti + 1) * 128],
                    ps[:, 2 * b:2 * b + 2, :].rearrange("p a (c t) -> p (a c) t", t=128), AF.Copy,
                    [("ps", 2 * b), ("ps", 2 * b + 1)], [("hTbo", tti // 4)])

            bank_rr = [0]

            def nbank(lo=0, n=4):
                b = lo + bank_rr[0] % n
                bank_rr[0] += 1
                return b
            for tix in range(15):
                wb = tix % 2
                wload(wst[wb], w_in_d[tix], ("wst", wb), stgA[wb], [("stgA", wb)])
                for tb in range(NB):
                    bk = nbank()
                    for c in range(8):
                        mm(ps[:, bk, :], wst[wb][:, c, :], hTb_old[:, c, cols(tb)], c == 0, c == 7,
                           [("wst", wb), ("hTbo", tb)], [("ps", bk)])
                    if tix < 8:
                        act(Qpair[:, tix, cols(tb)], ps[:, bk, :], AF.Copy, [("ps", bk)], [("Qpair", tix, tb)], scale=0.125)
                    elif tix < 10:
                        cp("dve", c_raw[:, tix - 8, cols(tb)], ps[:, bk, :], [("ps", bk)], [("c_raw", tb)])
                        act(csq[:, tix - 8, cols(tb)], ps[:, bk, :], AF.Square, [("ps", bk)], [("csq", tb)])
                    elif tix < 14:
                        act(q_idxT[:, tix - 10, cols(tb)], ps[:, bk, :], AF.Copy, [("ps", bk)], [("q_idxT",)])
                    else:
                        cp("dve", k_idxT[:, cols(tb)], ps[:, bk, :], [("ps", bk)], [("k_idxT",)])
            for tti in range(NT):
                for c in range(8):
                    mm(ps[:, 4, tti * 8:(tti + 1) * 8], hTb_old[:, c, tti * 128:(tti + 1) * 128], widx_w[:, c, :],
                       c == 0, c == 7, [("hTbo", tti // 4), "widx_w"], [("ps", 4)], skip=True)
            cp("dve", widx_sb[:], ps[:, 4, 0:128].rearrange("p (a b) -> p a b", a=16), [("ps", 4)], ["widx_sb"])

            sd_a = xs[0][:, 0:512]
            rstd_a = xs[1][:, 0:512]
            for tb in range(NB):
                bk = 5
                for cc in range(2):
                    mm(ps[:, bk, :], onesb[:], csq[:, cc, cols(tb)], cc == 0, cc == 1, ["onesb", ("csq", tb)], [("ps", bk)])
                act(sd_a, ps[:, bk, :], AF.Sqrt, [("ps", bk), "cst"], [("xs", 0)],
                    bias=cst[:, 0:1], scale=1.0 / 256)
                P.add("dve", lambda h: h.reciprocal(out=rstd_a, in_=sd_a), [("xs", 0)], [("xs", 1)])
                for cc in range(2):
                    stt("dve", c_kvT[:, cc, cols(tb)], c_raw[:, cc, cols(tb)], kvg[:, cc:cc + 1], rstd_a, ALU.mult, ALU.mult,
                        [("c_raw", tb), "kvg", ("xs", 1)], [("c_kvT", tb)])
            emit_phase()

            acc = [view(i * 8 * K, F32, [L]) for i in range(4)]
            A3O = 144 * K
            rbuf = [view(A3O + i * 2 * K, F32, [512]) for i in range(4)]
            junk = view(A3O + 8 * K, BF16, [L])
            mask_qs = [view(A3O + 12 * K + i * 4 * K, BF16, [L]) for i in range(2)]
            tbank = [0]
            F_LO, F_MX, F_W, F_HW, F_MID, F_CNT, F_STEP, F_NMID, F_S = range(9)
            junk2 = view(A3O + 20 * K, BF16, [L])

            def scf(f, c0=0, c1=4):
                return sc[:, f * 4 + c0:f * 4 + c1]
            rcount = 0
            for QB in range(NB):
                for jq in range(4):
                    qt = 4 * QB + jq
                    n = (qt + 1) * 128
                    a = acc[jq]
                    atok = ("acc", jq)
                    nsb = (n + 511) // 512
                    for sbk in range(nsb):
                        w = min(512, n - sbk * 512)
                        for hi in range(8):
                            r0 = (hi % 2) * 64
                            bk = nbank()
                            mm(ps[:, bk, 0:w], q_idxT[r0:r0 + 64, hi // 2, qt * 128:(qt + 1) * 128],
                               k_idxT[r0:r0 + 64, sbk * 512:sbk * 512 + w], True, True,
                               [("q_idxT",), ("k_idxT",)], [("ps", bk)])
                            rb = rcount % 4
                            rcount += 1
                            act(rbuf[rb][:, 0:w], ps[:, bk, 0:w], AF.Relu, [("ps", bk)], [("rbuf", rb)])
                            if hi == 0:
                                ts("dve", a[:, sbk * 512:sbk * 512 + w], rbuf[rb][:, 0:w], widx_sb[:, qt, 0:1], None, ALU.mult, None,
                                   [("rbuf", rb), "widx_sb"], [atok])
                            else:
                                stt("dve", a[:, sbk * 512:sbk * 512 + w], rbuf[rb][:, 0:w], widx_sb[:, qt, hi:hi + 1],
                                    a[:, sbk * 512:sbk * 512 + w], ALU.mult, ALU.add,
                                    [("rbuf", rb), "widx_sb", atok], [atok])
                    if qt >= 2:
                        P.add("dve", lambda h, a=a, qt=qt, jq=jq: h.tensor_reduce(out=scf(F_LO, jq, jq + 1), in_=a[:, 0:256],
                                                                                  axis=AX.X, op=ALU.min), [atok], ["sc_lo"])
                        P.add("dve", lambda h, a=a, n=n, jq=jq: h.tensor_reduce(out=scf(F_MX, jq, jq + 1), in_=a[:, 0:n],
                                                                                axis=AX.X, op=ALU.max), [atok], ["sc_mx"])
                    tt("dve", a[:, qt * 128:(qt + 1) * 128], a[:, qt * 128:(qt + 1) * 128], caus_add[:], ALU.add,
                       [atok, "caus_add", "sc_mx"], [atok])
                c0 = 2 if QB == 0 else 0
                tt("dve", scf(F_W, c0), scf(F_MX, c0), scf(F_LO, c0), ALU.subtract, ["sc_lo", "sc_mx"], ["sc_w"])
                act_cols = (1, 3) if c0 == 0 else ()
                for it in range(NIT):
                    f = 2.0 ** -(it + 1)
                    ts("dve", scf(F_HW, c0), scf(F_W, c0), f, None, ALU.mult, None, ["sc_w"], ["sc_hw"])
                    tt("dve", scf(F_MID, c0), scf(F_LO, c0), scf(F_HW, c0), ALU.add, ["sc_lo", "sc_hw"], ["sc_mid"])
                    if act_cols:
                        ts("dve", scf(F_NMID, c0), scf(F_MID, c0), -1.0, None, ALU.mult, None, ["sc_mid"], ["sc_nmid"])
                    for jq in act_cols:
                        n = (4 * QB + jq + 1) * 128
                        P.add("act", lambda h, jq=jq, n=n: h.activation(
                            out=junk2[:, 0:n], in_=acc[jq][:, 0:n], func=AF.Sign, bias=scf(F_NMID, jq, jq + 1), scale=1.0,
                            accum_out=scf(F_S, jq, jq + 1)), [("acc", jq), "sc_nmid"], ["junk2", ("sc_S", jq)])
                    for jq in range(c0, 4):
                        if jq in act_cols:
                            continue
                        n = (4 * QB + jq + 1) * 128
                        ts("dve", junk[:, 0:n], acc[jq][:, 0:n], scf(F_MID, jq, jq + 1), None, ALU.is_ge, ALU.add,
                           [("acc", jq), "sc_mid"], ["junk", "sc_cnt"], accum=scf(F_CNT, jq, jq + 1))
                    for jq in act_cols:
                        n = (4 * QB + jq + 1) * 128
                        ts("dve", scf(F_CNT, jq, jq + 1), scf(F_S, jq, jq + 1), 0.5, 0.5 * n, ALU.mult, ALU.add,
                           [("sc_S", jq)], ["sc_cnt"])
                    stt("dve", scf(F_STEP, c0), scf(F_CNT, c0), 255.5, scf(F_HW, c0), ALU.is_gt, ALU.mult,
                        ["sc_cnt", "sc_hw"], ["sc_step"])
                    tt("dve", scf(F_LO, c0), scf(F_LO, c0), scf(F_STEP, c0), ALU.add, ["sc_lo", "sc_step"], ["sc_lo"])
                for jq in range(4):
                    qt = 4 * QB + jq
                    n = (qt + 1) * 128
                    if qt < 2:
                        lo_ap, lo_tok = cst[:, 2:3], "cst"
                    else:
                        lo_ap, lo_tok = scf(F_LO, jq, jq + 1), "sc_lo"
                    mq = mask_qs[jq % 2]
                    mtok = ("mask_q", jq % 2)
                    ts("dve", mq[:, 0:n], acc[jq][:, 0:n], lo_ap, None, ALU.is_ge, None, [("acc", jq), lo_tok], [mtok])
                    for k0 in range(0, qt + 1, 4):
                        nk = min(4, qt + 1 - k0)
                        bk = 4 + tbank[0] % 2
                        tbank[0] += 1
                        for i in range(nk):
                            tr(psb(bk)[:, i * 128:(i + 1) * 128], mq[:, (k0 + i) * 128:(k0 + i + 1) * 128], identb[:],
                               [mtok, "identb"], [("ps", bk)])
                        act(maskT[:, MOFF[QB] + k0:MOFF[QB] + k0 + nk, jq * 128:(jq + 1) * 128],
                            psb(bk)[:, 0:nk * 128].rearrange("p (a b) -> p a b", a=nk), AF.Copy,
                            [("ps", bk)], [("maskT", QB)])
            emit_phase()

            BO = 144 * K
            KA = [view(BO + i * 4 * K, BF16, [L]) for i in range(4)]
            Vg = view(BO + 16 * K, BF16, [16, 4, 65])
            QA = [[view(BO + 26 * K + (b * 4 + i) * K, BF16, [512]) for i in range(4)] for b in range(2)]
            ptb = [view(BO + 34 * K + i * K, BF16, [512]) for i in range(8)]
            otok = [view(BO + 42 * K + i * 2 * K, BF16, [4, 256]) for i in range(2)]
            for i in range(4):
                if i % 2 == 1:
                    memset("pool", KA[i][0:64, :], 0.0, [("KA", i)])
                    dma("sp", KA[i][32:40, :], posrows_d, (), [("KA", i)])
                    for b in range(2):
                        memset("pool", QA[b][i][0:64, :], 0.0, [("QA", b, i)])
                else:
                    dma("sp", KA[i][64:72, :], posrows_d, (), [("KA", i)])
            memset("pool", Vg[:, :, :, 64:65], 1.0, ["Vg"])
            hcount = 0
            for g in range(4):
                for pr in range(2):
                    ptile = 2 * g + pr
                    for tb in range(NB):
                        bk = 4 + (pr * 4 + tb) % 2
                        for cc in range(2):
                            mm(ps[:, bk, :], wuk[:, cc, ptile * 128:(ptile + 1) * 128], c_kvT[:, cc, cols(tb)], cc == 0, cc == 1,
                               ["wuk", ("c_kvT", tb)], [("ps", bk)])
                        act(KA[2 * pr][0:64, cols(tb)], ps[0:64, bk, :], AF.Copy, [("ps", bk)], [("KA", 2 * pr)])
                        cp("dve", KA[2 * pr + 1][64:128, cols(tb)], ps[64:128, bk, :], [("ps", bk)], [("KA", 2 * pr + 1)])
                for s_t in range(NT):
                    bk = 4 + s_t % 2
                    for cc in range(2):
                        mm(ps[:, bk, 0:256], c_kvT[:, cc, s_t * 128:(s_t + 1) * 128], wuv[:, cc, g * 256:(g + 1) * 256],
                           cc == 0, cc == 1, ["wuv", ("c_kvT", s_t // 4)], [("ps", bk)])
                    src = ps[:, bk, 0:256].rearrange("p (a b) -> p a b", a=4)
                    if s_t % 2 == 0:
                        act(Vg[:, s_t, :, 0:64], src, AF.Copy, [("ps", bk)], ["Vg"])
                    else:
                        cp("dve", Vg[:, s_t, :, 0:64], src, [("ps", bk)], ["Vg"])
                items = [(QB, hl, kt) for QB in range(NB) for hl in range(4) for kt in range(4 * QB + 4)]
                LA = 5
                obank_of = {}
                pb_of = {}

                def front(i):
                    QB, hl, kt = items[i]
                    qb = (g * 4 + QB) % 2
                    if hl == 0 and kt == 0:
                        for h2 in range(4):
                            h_abs = 4 * g + h2
                            ptile = 2 * g + h2 // 2
                            if h2 % 2 == 0:
                                cp("pool", QA[qb][h2][0:64, :], Qpair[0:64, ptile, cols(QB)],
                                   [("Qpair", ptile, QB)], [("QA", qb, h2)])
                                dma("sp", QA[qb][h2][64:72, :], qcoef_d[h_abs, :, cols(QB)], (), [("QA", qb, h2)])
                            else:
                                cp("pool", QA[qb][h2][64:128, :], Qpair[64:128, ptile, cols(QB)],
                                   [("Qpair", ptile, QB)], [("QA", qb, h2)])
                                dma("sp", QA[qb][h2][32:40, :], qcoef_d[h_abs, :, cols(QB)], (), [("QA", qb, h2)])
                    kr = 72 if hl % 2 == 0 else 128
                    j0 = max(0, kt - 4 * QB)
                    sbank = nbank(0, 6)
                    mm(ps[:, sbank, j0 * 128:512], KA[hl][0:kr, kt * 128:(kt + 1) * 128], QA[qb][hl][0:kr, j0 * 128:512],
                       True, kt < 4 * QB, [("KA", hl), ("QA", qb, hl)], [("ps", sbank)])
                    if kt >= 4 * QB:
                        mm(ps[:, sbank, j0 * 128:(j0 + 1) * 128], identb[:], causT[:], False, True,
                           ["identb", "causT"], [("ps", sbank)])
                    pb = i % 8
                    pb_of[i] = pb
                    act(ptb[pb][:, j0 * 128:512], ps[:, sbank, j0 * 128:512], AF.Exp, [("ps", sbank)], [("ptb", pb)])
                    tt("dve", ptb[pb][:, j0 * 128:512], ptb[pb][:, j0 * 128:512],
                       maskT[:, MOFF[QB] + kt, j0 * 128:512], ALU.mult, [("ptb", pb), ("maskT", QB)], [("ptb", pb)])

                def back(i):
                    nonlocal hcount
                    QB, hl, kt = items[i]
                    ob = (g * 4 + QB) % 2
                    if kt == 0:
                        obank_of[(QB, hl)] = 6 + hcount % 2
                        hcount += 1
                    obank = obank_of[(QB, hl)]
                    psO = ps[:, obank, :].rearrange("p (j f) -> p j f", j=4)
                    j0 = max(0, kt - 4 * QB)
                    pb = pb_of[i]
                    for j in range(j0, 4):
                        mm(psO[:, j, 0:65], ptb[pb][:, j * 128:(j + 1) * 128], Vg[:, kt, hl, :],
                           kt == 0 and j == 0, kt == 4 * QB + j, [("ptb", pb), "Vg"], [("ps", obank)], skip=True)
                    if kt == 4 * QB + 3:
                        ri = obank - 6
                        rd = rden[:, ri, :]
                        ts("dve", rd, psO[:, :, 64], 1e-30, None, ALU.add, None, [("ps", obank)], [("rden", ri)])
                        P.add("dve", lambda h, rd=rd: h.reciprocal(out=rd, in_=rd), [("rden", ri)], [("rden", ri)])
                        for j in range(4):
                            ts("dve", otok[ob][:, j, hl * 64:(hl + 1) * 64], psO[:, j, 0:64], rden[:, ri, j:j + 1], None,
                               ALU.mult, None, [("ps", obank), ("rden", ri)], [("otok", ob)])
                        if hl == 3:
                            for cc in range(2):
                                bk = 4 + cc
                                for j in range(4):
                                    tr(psb(bk)[:, j * 128:(j + 1) * 128], otok[ob][:, j, cc * 128:(cc + 1) * 128], identb[:],
                                       [("otok", ob), "identb"], [("ps", bk)])
                                act(oT[:, 2 * g + cc, cols(QB)], psb(bk)[:, 0:512], AF.Copy, [("ps", bk)], [("oT", QB)])

                for step in range(len(items) + LA):
                    if step < len(items):
                        front(step)
                    if step - LA >= 0:
                        back(step - LA)
            emit_phase()

            hT = view(96 * K, F32, [8, L])
            hTb = view(0, BF16, [8, L])
            SO = 160 * K
            wst2 = [view(SO + i * 2 * K, BF16, [8, 128]) for i in range(2)]
            xs2 = [view(SO + 4 * K + i * 4 * K, F32, [D]) for i in range(2)]
            zb = [view(SO + 12 * K + i * K, BF16, [512]) for i in range(2)]
            zsq = [view(SO + 14 * K + i * K, BF16, [512]) for i in range(2)]
            mean_t = view(SO + 16 * K, F32, [512])
            msq_t = view(SO + 18 * K, F32, [512])
            rstd_t = view(SO + 20 * K, F32, [512])
            tbuf = [view(SO + 22 * K + i * 2 * K, F32, [512]) for i in range(2)]
            sgb = [view(SO + 26 * K + i * 2 * K, F32, [512]) for i in range(2)]
            actT = view(32 * K, BF16, [NJ, 1024])
            wdn = [view(76 * K + i * 6 * K, BF16, [NJ, 128]) for i in range(2)]
            wgu = [view(88 * K + i * 2 * K, BF16, [8, 128]) for i in range(4)]
            gst = [view(SO + i * 4 * K, F32, [8, 128]) for i in range(3)]
            dst_ = view(SO, F32, [NJ, 128])
            gcount = [0]
            lcount = [0]

            def layer_norm(tb, lyr, which, final_bf16=True):
                gi, bi = (0, 1) if which == 0 else (2, 3)
                for c in range(8):
                    k = lcount[0] % 2
                    lcount[0] += 1
                    cp("dve", zb[k], hT[:, c, cols(tb)], [("hT", c, tb)], [("zb", k)])
                    act(zsq[k], hT[:, c, cols(tb)], AF.Square, [("hT", c, tb)], [("zsq", k)])
                    mm(ps[:, 6, :], onesb[:], zb[k], c == 0, c == 7, ["onesb", ("zb", k)], [("ps", 6)])
                    mm(ps[:, 7, :], onesb[:], zsq[k], c == 0, c == 7, ["onesb", ("zsq", k)], [("ps", 7)])
                ts("dve", mean_t, ps[:, 6, :], 1.0 / D, None, ALU.mult, None, [("ps", 6)], ["mean_t"])
                tt("dve", msq_t, mean_t, mean_t, ALU.mult, ["mean_t"], ["msq_t"])
                stt("dve", msq_t, ps[:, 7, :], 1.0 / D, msq_t, ALU.mult, ALU.subtract, [("ps", 7), "msq_t"], ["msq_t"])
                act(rstd_t, msq_t, AF.Sqrt, ["msq_t", "cst"], ["rstd_t"], bias=cst[:, 1:2], scale=1.0)
                P.add("dve", lambda h: h.reciprocal(out=rstd_t, in_=rstd_t), ["rstd_t"], ["rstd_t"])
                for c in range(8):
                    k = lcount[0] % 2
                    lcount[0] += 1
                    tt("dve", tbuf[k], hT[:, c, cols(tb)], mean_t, ALU.subtract, [("hT", c, tb), "mean_t"], [("tbuf", k)])
                    tt("dve", tbuf[k], tbuf[k], rstd_t, ALU.mult, [("tbuf", k), "rstd_t"], [("tbuf", k)])
                    act(hT[:, c, cols(tb)], tbuf[k], AF.Identity, [("tbuf", k), "lnp"], [("hT", c, tb)],
                        bias=lnp[:, lyr, bi, c:c + 1], scale=lnp[:, lyr, gi, c:c + 1])
                    if final_bf16:
                        ts("pool", hTb[:, c, cols(tb)], tbuf[k], lnp[:, lyr, gi, c:c + 1], lnp[:, lyr, bi, c:c + 1],
                           ALU.mult, ALU.add, [("tbuf", k), "lnp"], [("hTb", c, tb)])

            for tti in range(NT):
                b = tti % 2
                dma("sp", xs2[b], x_d[tti * 128:(tti + 1) * 128, :], (), [("xs2", b)])
                for c in range(8):
                    tr(ps[:, 2 * b + c // 4, (c % 4) * 128:(c % 4 + 1) * 128], xs2[b][:, c * 128:(c + 1) * 128], ident[:],
                       [("xs2", b), "ident"], [("ps", 2 * b + c // 4)])
                act(hT[:, :, tti * 128:(tti + 1) * 128],
                    ps[:, 2 * b:2 * b + 2, :].rearrange("p a (c t) -> p (a c) t", t=128), AF.Copy,
                    [("ps", 2 * b), ("ps", 2 * b + 1)], [("hT", c, tti // 4) for c in range(8)], scale=ALPHA)

            def out_proj(w_d, src, srctok, wst2=wst2, stg=None, alpha=None):
                for it in range(8):
                    wb = it % 2
                    if stg is None:
                        wload(wst2[wb], w_d[it], ("wst3", wb), xs2[wb].rearrange("p (a b) -> p a b", a=8), [("xs2", wb)])
                    else:
                        wload(wst2[wb], w_d[it], ("wst3", wb), stg, ["stgE"])
                    for tb in range(NB):
                        bk = nbank(0, 4)
                        for c in range(8):
                            mm(ps[:, bk, :], wst2[wb][:, c, :], src[:, c, cols(tb)], c == 0, c == 7,
                               [("wst3", wb), (srctok, tb)], [("ps", bk)])
                        if alpha is None:
                            tt("dve", hT[:, it, cols(tb)], hT[:, it, cols(tb)], ps[:, bk, :], ALU.add,
                               [("ps", bk), ("hT", it, tb)], [("hT", it, tb)])
                        else:
                            stt("dve", hT[:, it, cols(tb)], hT[:, it, cols(tb)], alpha, ps[:, bk, :], ALU.mult, ALU.add,
                                [("ps", bk), ("hT", it, tb)], [("hT", it, tb)])
            out_proj(a_wo_d, oT, "oT")
            for tb in range(NB):
                layer_norm(tb, 0, 0)
            snapshot(hT)
            emit_phase()

            def ffn(lyr, last):
                def load_gu(jt):
                    wb = jt % 2
                    k0 = gcount[0] % 3
                    k1 = (gcount[0] + 1) % 3
                    gcount[0] += 2
                    wload(wgu[wb], w_gu_d[lyr, jt], ("wgu", wb), gst[k0], [("gst", k0)], ceng="act")
                    wload(wgu[2 + wb], w_gu_d[lyr, NJ + jt], ("wgu", 2 + wb), gst[k1], [("gst", k1)], ceng="pool")

                def load_dn(it):
                    wb = it % 2
                    toks = [("gst", 0), ("gst", 1), ("gst", 2)]
                    dma("sp", dst_, w_dn_d[lyr, it], (), toks)
                    act(wdn[wb][:, 0:11, :], dst_[:, 0:11, :], AF.Copy, toks, [("wdn", wb, 0)])
                    cp("pool", wdn[wb][:, 11:NJ, :], dst_[:, 11:NJ, :], toks, [("wdn", wb, 1)])

                for half in range(2):
                    load_gu(0)
                    for jt in range(NJ):
                        wb = jt % 2
                        if jt + 1 < NJ:
                            load_gu(jt + 1)
                        for t2 in range(2):
                            tb = 2 * half + t2
                            bg = nbank(0, 4)
                            bu = nbank(0, 4)
                            for c in range(8):
                                mm(ps[:, bg, :], wgu[wb][:, c, :], hTb[:, c, cols(tb)], c == 0, c == 7,
                                   [("wgu", wb), ("hTb", c, tb)], [("ps", bg)])
                            for c in range(8):
                                mm(ps[:, bu, :], wgu[2 + wb][:, c, :], hTb[:, c, cols(tb)], c == 0, c == 7,
                                   [("wgu", 2 + wb), ("hTb", c, tb)], [("ps", bu)])
                            k = (jt * 2 + t2) % 2
                            act(sgb[k], ps[:, bg, :], AF.Silu, [("ps", bg)], [("sgb", k)])
                            tt("dve", actT[:, jt, t2 * 512:(t2 + 1) * 512], sgb[k], ps[:, bu, :], ALU.mult,
                               [("sgb", k), ("ps", bu)], [("actT", t2)])
                    load_dn(0)
                    for it in range(8):
                        wb = it % 2
                        if it + 1 < 8:
                            load_dn(it + 1)
                        for t2 in range(2):
                            tb = 2 * half + t2
                            bk = nbank(0, 4)
                            for jt in range(NJ):
                                mm(ps[:, bk, :], wdn[wb][:, jt, :], actT[:, jt, t2 * 512:(t2 + 1) * 512], jt == 0, jt == NJ - 1,
                                   [("wdn", wb, 0), ("wdn", wb, 1), ("actT", t2)], [("ps", bk)])
                            stt("dve", hT[:, it, cols(tb)], hT[:, it, cols(tb)], ALPHA, ps[:, bk, :], ALU.mult, ALU.add,
                                [("ps", bk), ("hT", it, tb)], [("hT", it, tb)])
                    for t2 in range(2):
                        layer_norm(2 * half + t2, lyr, 1, final_bf16=not last)
                snapshot(hT)
                emit_phase()
            ffn(0, False)

            pooledT = view(32 * K, BF16, [8, L])
            ysT = view(64 * K, BF16, [8, L])
            UP = (16 + L) * 4
            upad = [view(64 * K + i * UP, F32, [16 + L]) for i in range(2)]
            sA = view(64 * K + 2 * UP, F32, [16 + L])
            sB = view(SO + 2 * UP, F32, [16 + L])
            wst3 = [view(SO + 3 * UP + i * 2 * K, BF16, [8, 128]) for i in range(2)]
            wgr = view(SO + 3 * UP + 4 * K, BF16, [2, 256])
            fix = view(SO + 3 * UP + 5 * K, F32, [16])
            stgE = view(SO + 3 * UP + 6 * K, F32, [8, 128])
            stgE2 = view(SO + 3 * UP + 6 * K, F32, [2, 256])
            for t_ in (upad[0], upad[1], sA, sB):
                memset("pool", t_[:, 0:16], 0.0, ["pads"])
            for mt in range(8):
                gi = mt // 2
                w = WINS[gi]
                wb = mt % 2
                u = upad[wb]
                utok = ("upad", wb)
                wload(wst3[wb], b_win_d[mt], ("wst3", wb), stgE, ["stgE"])
                for tb in range(NB):
                    bk = nbank(0, 4)
                    for c in range(8):
                        mm(ps[:, bk, :], wst3[wb][:, c, :], hTb[:, c, cols(tb)], c == 0, c == 7,
                           [("wst3", wb), ("hTb", c, tb)], [("ps", bk)])
                    act(u[:, 16 + tb * 512:16 + (tb + 1) * 512], ps[:, bk, :], AF.Copy, [("ps", bk), "pads"], [utok])
                tt("dve", sA[:, 16:], u[:, 16:], u[:, 15:15 + L], ALU.add, [utok, "pads"], ["sA"])
                cur, curtok = sA, "sA"
                if w >= 4:
                    tt("dve", sB[:, 16:], sA[:, 16:], sA[:, 14:14 + L], ALU.add, ["sA", "pads"], ["sB"])
                    cur, curtok = sB, "sB"
                if w >= 8:
                    tt("dve", sA[:, 16:], sB[:, 16:], sB[:, 12:12 + L], ALU.add, ["sB", "pads"], ["sA"])
                    cur, curtok = sA, "sA"
                if w >= 16:
                    tt("dve", sB[:, 16:], sA[:, 16:], sA[:, 8:8 + L], ALU.add, ["sA", "pads"], ["sB"])
                    cur, curtok = sB, "sB"
                stt("dve", pooledT[:, mt, :], cur[:, 16:], 1.0 / w, u[:, 16:], ALU.mult, ALU.subtract,
                    [curtok, utok], [("pooledT", mt)])
                tt("dve", fix[:, 0:16], cur[:, 16:32], invc[:, gi, :], ALU.mult, [curtok, "invc"], ["fix"])
                tt("dve", pooledT[:, mt, 0:16], fix[:, 0:16], u[:, 16:32], ALU.subtract, ["fix", utok], [("pooledT", mt)])
            emit_phase()
            for gi in range(4):
                wload(wgr, b_wgrp_d[gi], "wgr", stgE2, ["stgE"])
                for dt_ in range(2):
                    mo = 2 * gi + dt_
                    for tb in range(NB):
                        bk = nbank(0, 4)
                        for cc in range(2):
                            mm(ps[:, bk, :], wgr[:, cc, dt_ * 128:(dt_ + 1) * 128], pooledT[:, 2 * gi + cc, cols(tb)],
                               cc == 0, cc == 1, ["wgr", ("pooledT", 2 * gi + cc)], [("ps", bk)])
                        act(ysT[:, mo, cols(tb)], ps[:, bk, :], AF.Identity, [("ps", bk), "bscale"], [("ysT", tb)],
                            scale=bscale[:, mo:mo + 1])
            out_proj(b_wo_d, ysT, "ysT", wst2=wst3, stg=stgE, alpha=ALPHA)
            for tb in range(NB):
                layer_norm(tb, 1, 0)
            snapshot(hT)
            emit_phase()

            ffn(1, True)

            for tti in range(NT):
                b = tti % 2
                for c in range(8):
                    tr(ps[:, 2 * b + c // 4, (c % 4) * 128:(c % 4 + 1) * 128], hT[:, c, tti * 128:(tti + 1) * 128], ident[:],
                       [("hT", c, tti // 4), "ident"], [("ps", 2 * b + c // 4)])
                src = ps[:, 2 * b:2 * b + 2, :].rearrange("p a f -> p (a f)")
                if b == 0:
                    cp("dve", xs2[b], src, [("ps", 2 * b), ("ps", 2 * b + 1)], [("xs2", b)])
                else:
                    act(xs2[b], src, AF.Copy, [("ps", 2 * b), ("ps", 2 * b + 1)], [("xs2", b)])
                dma("sp", out_d[tti * 128:(tti + 1) * 128, :], xs2[b], [("xs2", b)], [("out", tti)])
            emit_phase()

        except _Stop:
            pass
    return nc


def _alibi_slopes():
    return np.exp2(-8.0 * np.arange(1, 17, dtype=np.float64) / 16.0)


def _host_consts():
    bf = ml_dtypes.bfloat16
    c = {}
    c["ident"] = np.eye(128, dtype=np.float32)
    c["identb"] = np.eye(128, dtype=np.float32).astype(bf)
    s = np.arange(L)
    pos = np.zeros((8, L), np.float32)
    pos[0] = s // 16
    pos[1] = s // 16
    pos[2] = s % 16
    pos[3] = s % 16
    pos[4] = 1.0
    c["posrows"] = pos.astype(bf)
    sl = _alibi_slopes()
    qc = np.zeros((16, 8, L), np.float32)
    for h in range(16):
        hi = np.float32(sl[h]).astype(bf).astype(np.float32)
        lo = np.float32(sl[h] - float(hi)).astype(bf).astype(np.float32)
        qc[h, 0] = 16.0 * hi
        qc[h, 1] = 16.0 * lo
        qc[h, 2] = hi
        qc[h, 3] = lo
        qc[h, 4] = -(sl[h] * s)
    c["qcoef"] = qc.astype(bf)
    qi = np.arange(128)[:, None]
    si = np.arange(128)[None, :]
    c["caus_add"] = np.where(si <= qi, 0.0, -1e30).astype(np.float32)
    c["causT"] = np.where(qi <= si, 0.0, -30000.0).astype(np.float32).astype(bf)
    invc = np.zeros((128, 4, 16), np.float32)
    for gi, w in enumerate(WINS):
        invc[:, gi, :] = 1.0 / np.minimum(w, np.arange(16) + 1)
    c["invc"] = invc
    return c


def _tiles_kc(w, ncols_tile=128):
    kd, n = w.shape
    return np.ascontiguousarray(w.reshape(kd // 128, 128, n // ncols_tile, ncols_tile).transpose(2, 1, 0, 3))


def _vec_pc(v):
    return np.ascontiguousarray(v.reshape(-1, 128).T)


def _prep_weights(a_w_in, a_w_uk, a_w_uv, a_kv_norm_g, a_w_o, b_w_in, b_w_grp, b_scale, b_w_o,
                  f_w_gu, f_w_down, ln_mix_g, ln_mix_b, ln_ffn_g, ln_ffn_b):
    m = {}
    w_in = a_w_in[0]
    colsel = np.concatenate([np.arange(0, 1792), np.arange(1792, 1856), np.arange(1792, 1856)])
    m["w_in_t"] = _tiles_kc(w_in[:, colsel])
    m["w_widx"] = np.ascontiguousarray(w_in[:, 1856:1864].reshape(8, 128, 8).transpose(1, 0, 2))
    m["w_uk_t"] = np.ascontiguousarray(a_w_uk[0].transpose(1, 0, 2).reshape(2, 128, 1024).transpose(1, 0, 2))
    m["w_uv_t"] = np.ascontiguousarray(a_w_uv[0].transpose(1, 0, 2).reshape(2, 128, 1024).transpose(1, 0, 2))
    m["kvg"] = _vec_pc(a_kv_norm_g[0])
    m["a_wo_t"] = _tiles_kc(a_w_o[0])
    m["b_win_t"] = _tiles_kc(b_w_in[0])
    m["b_wgrp_t"] = np.ascontiguousarray(b_w_grp[0].reshape(4, 2, 128, 256).transpose(0, 2, 1, 3))
    m["b_scale_t"] = _vec_pc(b_scale[0])
    m["b_wo_t"] = _tiles_kc(b_w_o[0])
    m["w_gu_t"] = np.stack([_tiles_kc(f_w_gu[i]) for i in range(2)])
    m["w_dn_t"] = np.stack([_tiles_kc(f_w_down[i]) for i in range(2)])
    lnp = np.zeros((128, 2, 4, 8), np.float32)
    for i in range(2):
        lnp[:, i, 0] = _vec_pc(ln_mix_g[i])
        lnp[:, i, 1] = _vec_pc(ln_mix_b[i])
        lnp[:, i, 2] = _vec_pc(ln_ffn_g[i])
        lnp[:, i, 3] = _vec_pc(ln_ffn_b[i])
    m["lnp"] = lnp
    return {k: np.ascontiguousarray(v, dtype=np.float32) for k, v in m.items()}


_NC_CACHE = {}


def kernel(x, a_w_in, a_w_uk, a_w_uv, a_kv_norm_g, a_w_o, b_w_in, b_w_grp, b_scale, b_w_o,
           f_w_gu, f_w_down, ln_mix_g, ln_mix_b, ln_ffn_g, ln_ffn_b, _debug=False, _stop=99, _ncores=8):
    f = lambda a: np.asarray(a, dtype=np.float32)
    wm = _prep_weights(f(a_w_in), f(a_w_uk), f(a_w_uv), f(a_kv_norm_g), f(a_w_o), f(b_w_in), f(b_w_grp),
                       f(b_scale), f(b_w_o), f(f_w_gu), f(f_w_down), f(ln_mix_g), f(ln_mix_b), f(ln_ffn_g), f(ln_ffn_b))
    wm.update(_host_consts())
    x = f(x)
    ncores = _ncores
    key = (_debug, _stop)
    if key not in _NC_CACHE:
        _NC_CACHE[key] = build_nc(debug=_debug, stop=_stop)
    nc = _NC_CACHE[key]
    in_maps = []
    for i in range(ncores):
        d = dict(wm)
        d["x"] = np.ascontiguousarray(x[i])
        in_maps.append(d)
    res = run_bass_kernel_spmd(nc, in_maps, core_ids=list(range(ncores)))
    out = np.stack([np.asarray(r["out"], dtype=np.float32) for r in res.results], axis=0)
    if _debug:
        return out, [r["dbg"] for r in res.results]
    return out
```
